# Optimizing a Trainium2 kernel written in Bass

```python
import jax, jax.numpy as jnp
from jax import lax
import numpy as np

D_MODEL = 1024
BATCH = 8
SEQ = 2048
DEPTH = 2

GRID_W = 64
CTX_LEN = 256
HEAD_DIM = 64
NA_HEADS = D_MODEL // 4 // HEAD_DIM
NA_WIN_R = 8
NA_WIN_C = 16
GQA_HEADS = D_MODEL // 2 // HEAD_DIM
GQA_KV_HEADS = GQA_HEADS // 4
GQA_BLOCK = 128
ROPE_THETA = 10000.0
HGRN_HEADS = D_MODEL // 4 // HEAD_DIM
HGRN_DK = HEAD_DIM
HGRN_DV = HEAD_DIM
HGRN_CHUNK = 16
N_GROUPS = 4
EXPERTS_PER_GROUP = 8
N_EXPERTS = N_GROUPS * EXPERTS_PER_GROUP
EXPERT_TOP_K = 2
EXPERT_FF = D_MODEL // 4
N_MOD = 6
EPS = 1e-6
NEG_INF = -1e30
LB_FLOOR = 1e-20

NA_W = NA_HEADS * HEAD_DIM
GQA_QW = GQA_HEADS * HEAD_DIM
GQA_KW = GQA_KV_HEADS * HEAD_DIM
HG_KW = HGRN_HEADS * HGRN_DK
HG_VW = HGRN_HEADS * HGRN_DV
D_MIX = NA_W + GQA_QW + HG_VW
IN_SPLIT_SIZES = (NA_W, NA_W, NA_W, GQA_QW, GQA_KW, GQA_KW, HG_KW, HG_VW, HG_KW, HG_KW, HG_VW)
D_IN = sum(IN_SPLIT_SIZES)

kernel_name = 'hybrid_na_gqa_hgrn2_hmoe_block'


def rms_norm(x, gain):
    xf = x.astype(jnp.float32)
    y = xf * lax.rsqrt(jnp.mean(xf * xf, axis=-1, keepdims=True) + EPS)
    return (y * gain.astype(jnp.float32)).astype(x.dtype)


def modulate(h, shift, scale):
    return h * (1 + scale) + shift


def to_heads(t, n):
    b, s, _ = t.shape
    return jnp.swapaxes(t.reshape(b, s, n, -1), 1, 2)


def from_heads(t):
    b, n, s, hd = t.shape
    return jnp.swapaxes(t, 1, 2).reshape(b, s, n * hd)


def split_cols(p):
    idx = np.cumsum(IN_SPLIT_SIZES)[:-1].tolist()
    return jnp.split(p, idx, axis=-1)


def axial_rope_tables(n_tokens):
    t = jnp.arange(n_tokens)
    row = (t // GRID_W).astype(jnp.float32)
    col = (t % GRID_W).astype(jnp.float32)
    half = HEAD_DIM // 2
    inv = ROPE_THETA ** (-jnp.arange(0, half, 2, dtype=jnp.float32) / half)
    ang = jnp.concatenate([row[:, None] * inv, col[:, None] * inv], axis=-1)
    return jnp.cos(ang), jnp.sin(ang)


def apply_rope(x, cos, sin):
    xf = x.astype(jnp.float32).reshape(*x.shape[:-1], HEAD_DIM // 2, 2)
    x1, x2 = xf[..., 0], xf[..., 1]
    out = jnp.stack([x1 * cos - x2 * sin, x1 * sin + x2 * cos], axis=-1)
    return out.reshape(x.shape).astype(x.dtype)


def softmax_attend(q, k, v):
    s = jnp.matmul(q, jnp.swapaxes(k, -1, -2)).astype(jnp.float32) * (q.shape[-1] ** -0.5)
    p = jax.nn.softmax(s, axis=-1).astype(v.dtype)
    return jnp.matmul(p, v)


def neighbourhood_attention(q, k, v, k_ctx, v_ctx, rpb):
    b, h, s, hd = q.shape
    rows = s // GRID_W
    wr = min(NA_WIN_R, rows)
    r = jnp.arange(rows)
    row_start = jnp.clip(r - wr // 2, 0, rows - wr)
    key_rows = row_start[:, None] + jnp.arange(wr)
    qg = q.reshape(b, h, rows, GRID_W, hd)
    kb = k.reshape(b, h, rows, GRID_W, hd)[:, :, key_rows].reshape(b, h, rows, wr * GRID_W, hd)
    vb = v.reshape(b, h, rows, GRID_W, hd)[:, :, key_rows].reshape(b, h, rows, wr * GRID_W, hd)
    cidx = jnp.arange(GRID_W)
    col_start = jnp.clip(cidx - NA_WIN_C // 2, 0, GRID_W - NA_WIN_C)
    col_ok = (cidx[None, :] >= col_start[:, None]) & (cidx[None, :] < col_start[:, None] + NA_WIN_C)
    dr = key_rows - r[:, None] + (NA_WIN_R - 1)
    dc = jnp.clip(cidx[None, :] - cidx[:, None] + (NA_WIN_C - 1), 0, 2 * NA_WIN_C - 2)
    bias = rpb.astype(jnp.float32)[:, dr[:, None, :, None], dc[None, :, None, :]]
    bias = jnp.where(col_ok[None, None, :, None, :], bias, NEG_INF).reshape(h, rows, GRID_W, wr * GRID_W)
    scale = hd ** -0.5
    s_nb = jnp.einsum('bhrqd,bhrkd->bhrqk', qg, kb).astype(jnp.float32) * scale + bias
    s_cx = jnp.einsum('bhrqd,bhld->bhrql', qg, k_ctx).astype(jnp.float32) * scale
    p = jax.nn.softmax(jnp.concatenate([s_nb, s_cx], axis=-1), axis=-1).astype(v.dtype)
    n_nb = wr * GRID_W
    out = (jnp.einsum('bhrqk,bhrkd->bhrqd', p[..., :n_nb], vb)
           + jnp.einsum('bhrql,bhld->bhrqd', p[..., n_nb:], v_ctx))
    return out.reshape(b, h, s, hd)


def gqa_block_sweep(q, k_all, v_all):
    b, hkv, g, s, hd = q.shape
    nb = s // GQA_BLOCK
    qb = jnp.moveaxis(q.reshape(b, hkv, g, nb, GQA_BLOCK, hd), 3, 0)
    ob = lax.map(lambda qi: softmax_attend(qi, k_all, v_all), qb)
    return jnp.moveaxis(ob, 0, 3).reshape(b, hkv, g, s, hd)


def hgrn_lower_bounds(lb_raw):
    p = jax.nn.softmax(lb_raw.astype(jnp.float32), axis=0)
    return jnp.cumsum(p, axis=0) - p[0]


def log_forget(z, lb):
    return jnp.logaddexp(jnp.log1p(-lb) + jax.nn.log_sigmoid(z), jnp.log(jnp.maximum(lb, LB_FLOOR)))


def hgrn2_chunkwise(q, k, v, logf, s0):
    b, h, t, dk = q.shape
    dv = v.shape[-1]
    c = HGRN_CHUNK
    n = t // c
    q, k, logf = (a.reshape(b, h, n, c, dk) for a in (q, k, logf))
    v = v.reshape(b, h, n, c, dv)
    cum = jnp.cumsum(logf, axis=3)
    lower = jnp.tril(jnp.ones((c, c), dtype=bool))[:, :, None]
    diff = cum[..., :, None, :] - cum[..., None, :, :]
    decay = jnp.where(lower, jnp.exp(jnp.where(lower, diff, 0.0)), 0.0)
    attn = jnp.einsum('bhntd,bhnsd,bhntsd->bhnts', q, k, decay)
    o_intra = jnp.einsum('bhnts,bhnsv->bhntv', attn, v)
    cum_last = cum[..., -1:, :]
    u = jnp.einsum('bhnsd,bhnsv->bhndv', k * jnp.exp(cum_last - cum), v)
    a = jnp.exp(cum_last[..., 0, :])

    def step(s, xs):
        a_n, u_n = xs
        return a_n[..., None] * s + u_n, s

    s_fin, s_enter = lax.scan(step, s0, (jnp.moveaxis(a, 2, 0), jnp.moveaxis(u, 2, 0)))
    s_enter = jnp.moveaxis(s_enter, 0, 2)
    o_inter = jnp.einsum('bhntd,bhndv->bhntv', q * jnp.exp(cum), s_enter)
    return (o_intra + o_inter).reshape(b, h, t, dv), s_fin


def hgrn2_final_state(k, v, logf):
    cum = jnp.cumsum(logf, axis=2)
    w = k * jnp.exp(cum[:, :, -1:, :] - cum)
    return jnp.einsum('bhtd,bhtv->bhdv', w, v)


def hgrn2_mixer(q, i, f_fwd, f_bwd, g, qc, ic, fc_fwd, fc_bwd, gc, lb, o_norm, ctx_out):
    dt = q.dtype
    nh = HGRN_HEADS
    bsz = q.shape[0]
    scale = HGRN_DK ** -0.5
    h32 = lambda t: to_heads(t, nh).astype(jnp.float32)
    rev = lambda t: t[:, :, ::-1]
    qh, vh = h32(q) * scale, h32(i)
    qch, vch = h32(qc) * scale, h32(ic)
    s_zero = jnp.zeros((bsz, nh, HGRN_DK, HGRN_DV), jnp.float32)
    o_lat, o_ctx = [], []
    for d, (f_l, f_c) in enumerate(((f_fwd, fc_fwd), (f_bwd, fc_bwd))):
        lbd = lb[d].reshape(nh, 1, HGRN_DK)
        logf = log_forget(h32(f_l), lbd)
        logfc = log_forget(h32(f_c), lbd)
        seq_l = (qh, -jnp.expm1(logf), vh, logf)
        seq_c = (qch, -jnp.expm1(logfc), vch, logfc)
        if d == 1:
            seq_l = tuple(rev(t) for t in seq_l)
            seq_c = tuple(rev(t) for t in seq_c)
        if ctx_out:
            oc, s_c = hgrn2_chunkwise(seq_c[0], seq_c[1], seq_c[2], seq_c[3], s_zero)
            o_ctx.append(rev(oc) if d == 1 else oc)
        else:
            s_c = hgrn2_final_state(seq_c[1], seq_c[2], seq_c[3])
        o, _ = hgrn2_chunkwise(seq_l[0], seq_l[1], seq_l[2], seq_l[3], s_c)
        o_lat.append(rev(o) if d == 1 else o)

    def readout(o_sum, gate):
        y = from_heads(rms_norm(o_sum, o_norm))
        return (y * jax.nn.silu(gate.astype(jnp.float32))).astype(dt)

    lat = readout(o_lat[0] + o_lat[1], g)
    ctx_o = readout(o_ctx[0] + o_ctx[1], gc) if ctx_out else None
    return lat, ctx_o


def token_mixers(h, hc, w_in, na_qn, na_kn, na_rpb, gq_qn, gq_kn, lb, hg_on, cos, sin, ctx_out):
    b, s, _ = h.shape
    L = hc.shape[1]
    grp = GQA_HEADS // GQA_KV_HEADS
    a_q, a_k, a_v, b_q, b_k, b_v, c_q, c_i, c_ff, c_fb, c_g = split_cols(h @ w_in)
    ac_q, ac_k, ac_v, bc_q, bc_k, bc_v, cc_q, cc_i, cc_ff, cc_fb, cc_g = split_cols(hc @ w_in)
    qa = rms_norm(to_heads(a_q, NA_HEADS), na_qn)
    ka = rms_norm(to_heads(a_k, NA_HEADS), na_kn)
    va = to_heads(a_v, NA_HEADS)
    kac = rms_norm(to_heads(ac_k, NA_HEADS), na_kn)
    vac = to_heads(ac_v, NA_HEADS)
    o_a = neighbourhood_attention(qa, ka, va, kac, vac, na_rpb)
    qb = apply_rope(rms_norm(to_heads(b_q, GQA_HEADS), gq_qn), cos, sin)
    kb = apply_rope(rms_norm(to_heads(b_k, GQA_KV_HEADS), gq_kn), cos, sin)
    vb = to_heads(b_v, GQA_KV_HEADS)
    kbc = rms_norm(to_heads(bc_k, GQA_KV_HEADS), gq_kn)
    vbc = to_heads(bc_v, GQA_KV_HEADS)
    k_all = jnp.concatenate([kb, kbc], axis=2)[:, :, None]
    v_all = jnp.concatenate([vb, vbc], axis=2)[:, :, None]
    o_b = gqa_block_sweep(qb.reshape(b, GQA_KV_HEADS, grp, s, HEAD_DIM), k_all, v_all)
    o_b = o_b.reshape(b, GQA_HEADS, s, HEAD_DIM)
    o_c, o_cc = hgrn2_mixer(c_q, c_i, c_ff, c_fb, c_g, cc_q, cc_i, cc_ff, cc_fb, cc_g, lb, hg_on, ctx_out)
    mix = jnp.concatenate([from_heads(o_a), from_heads(o_b), o_c], axis=-1)
    if not ctx_out:
        return mix, None
    qac = rms_norm(to_heads(ac_q, NA_HEADS), na_qn)
    o_ac = softmax_attend(qac, kac, vac)
    qbc = rms_norm(to_heads(bc_q, GQA_HEADS), gq_qn).reshape(b, GQA_KV_HEADS, grp, L, HEAD_DIM)
    o_bc = softmax_attend(qbc, kbc[:, :, None], vbc[:, :, None]).reshape(b, GQA_HEADS, L, HEAD_DIM)
    mix_c = jnp.concatenate([from_heads(o_ac), from_heads(o_bc), o_cc], axis=-1)
    return mix, mix_c


def hier_moe(t, w_rg, b_rg, w_re, b_re, w_gate, w_up, w_down):
    g_prob = jax.nn.softmax((t @ w_rg).astype(jnp.float32) + b_rg, axis=-1)
    g_top, g_idx = lax.top_k(g_prob, 1)
    e_logits = ((t @ w_re).astype(jnp.float32) + b_re).reshape(-1, N_GROUPS, EXPERTS_PER_GROUP)
    in_group = jnp.take_along_axis(e_logits, g_idx[:, :, None], axis=1)[:, 0]
    e_top, e_idx = lax.top_k(in_group, EXPERT_TOP_K)
    e_w = jax.nn.softmax(e_top, axis=-1) * g_top
    expert_id = g_idx * EXPERTS_PER_GROUP + e_idx
    combine = jnp.sum(jax.nn.one_hot(expert_id, N_EXPERTS, dtype=jnp.float32) * e_w[..., None], axis=1)
    combine = combine.reshape(-1, N_GROUPS, EXPERTS_PER_GROUP).astype(t.dtype)
    wg = w_gate.reshape(N_GROUPS, EXPERTS_PER_GROUP, D_MODEL, EXPERT_FF)
    wu = w_up.reshape(N_GROUPS, EXPERTS_PER_GROUP, D_MODEL, EXPERT_FF)
    wd = w_down.reshape(N_GROUPS, EXPERTS_PER_GROUP, EXPERT_FF, D_MODEL)
    out = jnp.zeros_like(t)
    for gi in range(N_GROUPS):
        hid = jax.nn.silu(jnp.einsum('td,edf->tef', t, wg[gi])) * jnp.einsum('td,edf->tef', t, wu[gi])
        out = out + jnp.einsum('tef,efd->td', hid * combine[:, gi, :, None], wd[gi])
    return out


def setup_inputs(seed: int = 0) -> dict:
    key = jax.random.key(seed)
    ks = jax.random.split(key, 32)
    nrm = lambda k, shape, sc: jax.random.normal(k, shape, jnp.float32) * sc
    D = D_MODEL
    return {
        'x': nrm(ks[0], (BATCH, SEQ, D), 1.0),
        'c': nrm(ks[1], (BATCH, D), 1.0),
        'ctx': nrm(ks[2], (BATCH, CTX_LEN, D), 1.0),
        'c_ctx': nrm(ks[3], (D,), 1.0),
        'w_ada': nrm(ks[4], (DEPTH, D, N_MOD * D), 0.5 * D ** -0.5),
        'b_ada': nrm(ks[5], (DEPTH, N_MOD * D), 0.02),
        'norm1_g': 1.0 + nrm(ks[6], (DEPTH, D), 0.02),
        'w_in': nrm(ks[7], (DEPTH, D, D_IN), D ** -0.5),
        'na_q_norm': 1.0 + nrm(ks[8], (DEPTH, HEAD_DIM), 0.02),
        'na_k_norm': 1.0 + nrm(ks[9], (DEPTH, HEAD_DIM), 0.02),
        'na_rpb': nrm(ks[10], (DEPTH, NA_HEADS, 2 * NA_WIN_R - 1, 2 * NA_WIN_C - 1), 0.5),
        'gqa_q_norm': 1.0 + nrm(ks[11], (DEPTH, HEAD_DIM), 0.02),
        'gqa_k_norm': 1.0 + nrm(ks[12], (DEPTH, HEAD_DIM), 0.02),
        'hgrn_lb': nrm(ks[13], (DEPTH, 2, HG_KW), 1.0),
        'hgrn_o_norm': 1.0 + nrm(ks[14], (DEPTH, HGRN_DV), 0.02),
        'w_out': nrm(ks[15], (DEPTH, D_MIX, D), D_MIX ** -0.5),
        'norm2_g': 1.0 + nrm(ks[16], (DEPTH, D), 0.02),
        'w_route_group': nrm(ks[17], (DEPTH, D, N_GROUPS), D ** -0.5),
        'b_route_group': nrm(ks[18], (DEPTH, N_GROUPS), 0.01),
        'w_route_expert': nrm(ks[19], (DEPTH, D, N_EXPERTS), D ** -0.5),
        'b_route_expert': nrm(ks[20], (DEPTH, N_EXPERTS), 0.01),
        'w_exp_gate': nrm(ks[21], (DEPTH, N_EXPERTS, D, EXPERT_FF), D ** -0.5),
        'w_exp_up': nrm(ks[22], (DEPTH, N_EXPERTS, D, EXPERT_FF), D ** -0.5),
        'w_exp_down': nrm(ks[23], (DEPTH, N_EXPERTS, EXPERT_FF, D), EXPERT_FF ** -0.5),
    }


def reference(x, c, ctx, c_ctx, w_ada, b_ada, norm1_g, w_in, na_q_norm, na_k_norm, na_rpb,
              gqa_q_norm, gqa_k_norm, hgrn_lb, hgrn_o_norm, w_out, norm2_g, w_route_group,
              b_route_group, w_route_expert, b_route_expert, w_exp_gate, w_exp_up, w_exp_down):
    b, s, d = x.shape
    L = ctx.shape[1]
    cos, sin = axial_rope_tables(s)
    lb_all = hgrn_lower_bounds(hgrn_lb)
    xc = ctx
    for layer in range(DEPTH):
        ctx_out = layer < DEPTH - 1
        mod = (jax.nn.silu(c) @ w_ada[layer] + b_ada[layer]).reshape(b, N_MOD, d)
        mod_c = (jax.nn.silu(c_ctx) @ w_ada[layer] + b_ada[layer]).reshape(N_MOD, d)
        h = modulate(rms_norm(x, norm1_g[layer]), mod[:, 0:1], mod[:, 1:2])
        hc = modulate(rms_norm(xc, norm1_g[layer]), mod_c[0], mod_c[1])
        mix, mix_c = token_mixers(h, hc, w_in[layer], na_q_norm[layer], na_k_norm[layer], na_rpb[layer],
                                  gqa_q_norm[layer], gqa_k_norm[layer], lb_all[layer], hgrn_o_norm[layer],
                                  cos, sin, ctx_out)
        x = x + mod[:, 2:3] * (mix @ w_out[layer])
        moe_args = (w_route_group[layer], b_route_group[layer], w_route_expert[layer],
                    b_route_expert[layer], w_exp_gate[layer], w_exp_up[layer], w_exp_down[layer])
        h2 = modulate(rms_norm(x, norm2_g[layer]), mod[:, 3:4], mod[:, 4:5])
        if ctx_out:
            xc = xc + mod_c[2] * (mix_c @ w_out[layer])
            hc2 = modulate(rms_norm(xc, norm2_g[layer]), mod_c[3], mod_c[4])
            ff = hier_moe(jnp.concatenate([h2.reshape(-1, d), hc2.reshape(-1, d)], axis=0), *moe_args)
            x = x + mod[:, 5:6] * ff[: b * s].reshape(b, s, d)
            xc = xc + mod_c[5] * ff[b * s:].reshape(b, L, d)
        else:
            x = x + mod[:, 5:6] * hier_moe(h2.reshape(-1, d), *moe_args).reshape(b, s, d)
    return x
```

```python
import contextlib
import numpy as np
import concourse.bass as bass
import concourse.mybir as mybir
from concourse.bass_utils import run_bass_kernel_spmd

F32 = mybir.dt.float32
BF16 = mybir.dt.bfloat16
AF = mybir.ActivationFunctionType
ALU = mybir.AluOpType
AX = mybir.AxisListType

NL = 2
D = 1024
SL = 2048
LC = 256
T = SL + LC
EPS = 1e-6
BLK = [(0, 512), (512, 1024), (1024, 1536), (1536, 2048), (2048, 2304)]
ENGS = ('pe', 'act', 'dve', 'pool', 'sp')


class Sched:
    def __init__(self, nc):
        self.nc = nc
        self.ops = []
        self.last_w = {}
        self.readers = {}
        self.dma_streams = []

    def op(self, eng, fn, r=(), w=(), dma=None):
        deps = set()
        for k in r:
            lw = self.last_w.get(k)
            if lw is not None:
                deps.add(lw)
        for k in w:
            lw = self.last_w.get(k)
            if lw is not None:
                deps.add(lw)
            deps.update(self.readers.get(k, ()))
        i = len(self.ops)
        if dma is not None:
            stream = ('d', dma)
            if stream not in self.dma_streams:
                self.dma_streams.append(stream)
        else:
            stream = ('e', eng)
        self.ops.append(dict(eng=eng, fn=fn, deps=deps, stream=stream))
        for k in r:
            self.readers.setdefault(k, []).append(i)
        for k in w:
            self.last_w[k] = i
            self.readers[k] = []
        return i

    def barrier(self):
        last = {}
        for i, o in enumerate(self.ops):
            last[o['stream']] = i
        ids = set(last.values())
        for e in ENGS:
            i = self.op(e, None)
            self.ops[i]['deps'] = set(ids)
        self.last_w = {}
        self.readers = {}

    def emit(self):
        nc = self.nc
        ops = self.ops
        spos = {}
        for o in ops:
            s = o['stream']
            o['pos'] = spos.get(s, 0)
            spos[s] = o['pos'] + 1
        known = {e: {} for e in ENGS}
        last_in_stream = {}
        for i, o in enumerate(ops):
            E = o['eng']
            kn = known[E]
            waits = []
            for d in sorted(o['deps']):
                Dd = ops[d]
                sD = Dd['stream']
                if sD == ('e', 'pe') and E == 'pe' and o['stream'][0] == 'e':
                    continue
                if kn.get(sD, -1) >= Dd['pos']:
                    continue
                if sD[0] == 'd':
                    Dd = ops[last_in_stream[sD]]
                Dd['sig'] = True
                waits.append(Dd)
                for s, p in Dd['clock'].items():
                    if kn.get(s, -1) < p:
                        kn[s] = p
            o['waits'] = waits
            ck = dict(kn)
            ck[o['stream']] = o['pos']
            o['clock'] = ck
            last_in_stream[o['stream']] = i
            if o['stream'][0] == 'd':
                o['sig'] = True
        cnt = {}
        for o in ops:
            s = o['stream']
            if o.get('sig'):
                inc = 16 if s[0] == 'd' else 1
                cnt[s] = cnt.get(s, 0) + inc
                o['sigval'] = cnt[s]
                o['inc'] = inc
        self.stats = dict(n_ops=len(ops), sig=dict(cnt))
        streams = [('e', e) for e in ENGS] + self.dma_streams
        with contextlib.ExitStack() as st:
            sems = {}
            for s in streams:
                sems[s] = st.enter_context(nc.semaphore("s_%s_%s" % (s[0], str(s[1]))))
            block = st.enter_context(nc.Block())

            def run(E):
                def body(eobj):
                    for o in ops:
                        if o['eng'] != E:
                            continue
                        best = {}
                        for Dd in o['waits']:
                            s = Dd['stream']
                            if best.get(s, 0) < Dd['sigval']:
                                best[s] = Dd['sigval']
                        for s, v in best.items():
                            eobj.wait_ge(sems[s], v)
                        if o['fn'] is None:
                            if o.get('sig'):
                                eobj.nop().then_inc(sems[o['stream']], o['inc'])
                            continue
                        ins = o['fn'](eobj)
                        if o.get('sig'):
                            ins.then_inc(sems[o['stream']], o['inc'])
                return body

            block.tensor(run('pe'))
            block.scalar(run('act'))
            block.vector(run('dve'))
            block.gpsimd(run('pool'))
            block.sync(run('sp'))


class _Stop(Exception):
    pass


def build_nc(n_layers=NL, dbg=(), stop=None):
    nc = bass.Bass("TRN2", target_bir_lowering=False)
    S = Sched(nc)

    def din(name, shape, dt=F32):
        return nc.dram_tensor(name, list(shape), dt, kind="ExternalInput").ap()

    xT_in = din("xT_in", [D, T])
    cvec_d = din("cvec", [128, 16])
    cbf_d = din("cbf", [128, 768])
    cf32_d = din("cf32", [128, 2112])
    sel_d = din("sel", [32, 4096])
    rope_d = din("rope", [128, 2, SL])
    w_ada_d = din("w_ada", [NL, D, 6 * D])
    b_ada_d = din("b_adaT", [NL, 128, 48])
    vecs_d = din("vecs", [NL, 128, 21])
    lbraw_d = din("lbraw", [128, 8])
    w_in_d = din("w_in", [NL, D, 2816])
    nab_d = din("nab", [NL, 128, 3840])
    w_out_d = din("w_out", [NL, D, D])
    wr_d = din("wr", [NL, D, 36])
    brb_d = din("brb", [NL, 128, 36])
    weg_d = din("w_eg", [NL, 32, 128, 2048])
    weu_d = din("w_eu", [NL, 32, 128, 2048])
    wed_d = din("w_ed", [NL, 32, 128, 2048])
    outT = nc.dram_tensor("outT", [D, SL], F32, kind="ExternalOutput").ap()
    hT_d = nc.dram_tensor("hT_scr", [5, 128, 4096], BF16, kind="Internal").ap()
    comb_d = nc.dram_tensor("comb_scr", [32, T], F32, kind="Internal").ap()
    dbg_out = {}

    st = contextlib.ExitStack()

    def sb(name, shape, dt=F32):
        return st.enter_context(nc.sbuf_tensor("s_" + name, list(shape), dt))

    psf = [st.enter_context(nc.psum_tensor("psf%d" % i, [128, 512], F32)) for i in range(8)]
    psb = [psf[6][:].bitcast(BF16), psf[7][:].bitcast(BF16)]
    ring_state = {'i': 0}

    def nextps():
        i = ring_state['i']
        ring_state['i'] = (i + 1) % 4
        return psf[i], 'psf%d' % i

    rings = {}

    def ring(name, n):
        i = rings.get(name, 0)
        rings[name] = (i + 1) % n
        return i

    XT = sb("XT", [128, 8, T])
    cbf = sb("cbf", [128, 768], BF16)
    cf32 = sb("cf32", [128, 2112])
    cvec = sb("cvec", [128, 8, 2])
    scv = sb("scv", [128, 8, 2])
    modT = sb("modT", [128, 48, 2])
    vecs = sb("vecs", [128, 21])
    A1 = sb("A1", [128, 8, 2])
    A2 = sb("A2", [128, 8, 2])
    lbs = sb("lbs", [128, 8])
    lbv = sb("lbv", [128, 8])
    onesf = sb("onesf", [128, 64])

    ident_bf = cbf[:, 0:128]
    swap_bf = cbf[:, 128:256]
    onesbd_bf = cbf[:, 256:384]
    ones_bf = cbf[:, 384:512]
    oneshalf = [cbf[:, 512:640], cbf[:, 640:768]]
    ident_f = cf32[:, 0:128]
    colmask = cf32[:, 128:192]
    Mdir = [cf32[:, 192:320], cf32[:, 320:448]]
    bdm = cf32[:, 448:576]
    cmfull = cf32[:, 576:1600]
    scanmask = cf32[:, 1600:2112]

    def mm(out, lhsT, rhs, start, stop, r, w):
        S.op('pe', lambda e: e.matmul(out, lhsT, rhs, start=start, stop=stop), r=r, w=w)

    def act(out, in_, func, r, w, scale=1.0, bias=0.0):
        S.op('act', lambda e: e.activation(out=out, in_=in_, func=func, bias=bias, scale=scale), r=r, w=w)

    def tt(eng, out, in0, in1, op, r, w):
        S.op(eng, lambda e: e.tensor_tensor(out=out, in0=in0, in1=in1, op=op), r=r, w=w)

    def ts(eng, out, in0, s1, s2, op0, op1, r, w):
        if s2 is None:
            S.op(eng, lambda e: e.tensor_scalar(out=out, in0=in0, scalar1=s1, scalar2=None, op0=op0), r=r, w=w)
        else:
            S.op(eng, lambda e: e.tensor_scalar(out=out, in0=in0, scalar1=s1, scalar2=s2, op0=op0, op1=op1), r=r, w=w)

    def stt(eng, out, in0, scalar, in1, op0, op1, r, w):
        S.op(eng, lambda e: e.scalar_tensor_tensor(out=out, in0=in0, scalar=scalar, in1=in1, op0=op0, op1=op1), r=r, w=w)

    def cp(eng, out, in_, r, w):
        if eng == 'act':
            act(out, in_, AF.Copy, r, w)
        else:
            S.op(eng, lambda e: e.tensor_copy(out=out, in_=in_), r=r, w=w)

    def recip(out, in_, r, w):
        S.op('dve', lambda e: e.reciprocal(out=out, in_=in_), r=r, w=w)

    def memset(eng, ap, val, w):
        S.op(eng, lambda e: e.memset(ap, val), w=w)

    def dma(eng, out, in_, r, w, stream):
        S.op(eng, lambda e: e.dma_start(out=out, in_=in_), r=r, w=w, dma=stream)

    def dump(name, ap, keys, shape, dt=F32):
        if name not in dbg:
            return
        d = nc.dram_tensor("dbg_" + name, list(shape), dt, kind="ExternalOutput").ap()
        dbg_out[name] = d
        dma('sp', d, ap, keys, ['dbg_' + name], 'dbg')

    dma('pool', cbf[:], cbf_d, [], ['cbf'], 'c0')
    dma('sp', cf32[:], cf32_d, [], ['cf32'], 'c1')
    dma('sp', cvec[:], cvec_d.rearrange("p (k j) -> p k j", j=2), [], ['cvec'], 'c2')
    dma('sp', lbs[:], lbraw_d, [], ['lbs'], 'c3')
    memset('pool', onesf[:], 1.0, ['onesf'])
    xv = xT_in.rearrange("(c p) t -> p c t", p=128)
    for c in range(8):
        dma('sp', XT[:, c, :], xv[:, c, :], [], [('XT', b) for b in range(5)], 'x%d' % (c % 4))
    act(scv[:], cvec[:], AF.Exp, ['cvec'], ['scv'], scale=-1.0)
    ts('dve', scv[:], scv[:], 1.0, None, ALU.add, None, ['scv'], ['scv'])
    recip(scv[:], scv[:], ['scv'], ['scv'])
    tt('dve', scv[:], scv[:], cvec[:], ALU.mult, ['scv', 'cvec'], ['scv'])
    lb1 = sb("lb1", [128, 4])
    tt('dve', lb1[:], lbs[:, 0:4], lbs[:, 4:8], ALU.subtract, ['lbs'], ['lb1'])
    act(lb1[:], lb1[:], AF.Exp, ['lb1'], ['lb1'])
    ts('dve', lb1[:], lb1[:], 1.0, None, ALU.add, None, ['lb1'], ['lb1'])
    recip(lb1[:], lb1[:], ['lb1'], ['lb1'])

    XTk = lambda b: [('XT', b)]

    def stage(k):
        if stop == k:
            raise _Stop()

    def layer(l):
        last = (l == NL - 1)
        nblk = 4 if last else 5
        S.barrier()
        dma('sp', vecs[:], vecs_d[l], [], ['vecs'], 'c2')
        if l == 0:
            memset('pool', lbv[:, 0:4], 1.0, ['lbv'])
            memset('pool', lbv[:, 4:8], 1e-20, ['lbv'])
        else:
            ts('dve', lbv[:, 0:4], lb1[:], -1.0, 1.0, ALU.mult, ALU.add, ['lb1'], ['lbv'])
            ts('dve', lbv[:, 4:8], lb1[:], 1e-20, None, ALU.max, None, ['lb1'], ['lbv'])
        with contextlib.ExitStack() as st2:
            wa = [st2.enter_context(nc.sbuf_tensor("wa%d_%d" % (i, l), [128, 8, 1024], F32)) for i in range(2)]
            badT = st2.enter_context(nc.sbuf_tensor("badT_%d" % l, [128, 48], F32))
            dma('sp', badT[:], b_ada_d[l], [], ['badT'], 'c3')
            wav = w_ada_d[l].rearrange("(kc p) n -> p kc n", p=128)
            psm = psf[4]
            for m in range(6):
                wt = wa[m % 2]
                dma('sp', wt[:], wav[:, :, m * 1024:(m + 1) * 1024], [], ['wa%d' % (m % 2)], 'wa%d' % (m % 2))
                for j in range(8):
                    for kc in range(8):
                        mm(psm[:, (m * 8 + j) * 2:(m * 8 + j) * 2 + 2], wt[:, kc, j * 128:(j + 1) * 128], scv[:, kc, :],
                           kc == 0, kc == 7, ['wa%d' % (m % 2), 'scv'], ['psm'])
            tt('dve', modT[:], psm[:, 0:96].rearrange("p (a b) -> p a b", b=2),
               badT[:].unsqueeze(2).to_broadcast([128, 48, 2]), ALU.add, ['psm', 'badT'], ['modT'])
        S.barrier()
        stage(0)
        stt('dve', A1[:], modT[:, 8:16, :], 1.0, vecs[:, 0:8].unsqueeze(2).to_broadcast([128, 8, 2]),
            ALU.add, ALU.mult, ['modT', 'vecs'], ['A1'])
        stt('dve', A2[:], modT[:, 32:40, :], 1.0, vecs[:, 8:16].unsqueeze(2).to_broadcast([128, 8, 2]),
            ALU.add, ALU.mult, ['modT', 'vecs'], ['A2'])

        def modv(m, c, col):
            return modT[:, m * 8 + c, col:col + 1]

        stm = contextlib.ExitStack()
        U = {}
        wph = stm.enter_context(nc.sbuf_tensor("wph_%d" % l, [128, 8, 640], BF16))

        def unit_bufs(st_, tag, nhb, nch):
            U['hb'] = [st_.enter_context(nc.sbuf_tensor("hb%d_%s" % (i, tag), [128, 8, 512], BF16)) for i in range(nhb)]
            U['woutu'] = st_.enter_context(nc.sbuf_tensor("woutu_%s" % tag, [128, nch, D], BF16))
            U['mixu'] = st_.enter_context(nc.sbuf_tensor("mixu_%s" % tag, [128, nch, T], BF16))
            rings['hb'] = 0
            pend_h.clear()

        def norm_block(b, Asc, mshift, tmp, sqb, rstd, sfx='', sfx_sq=None):
            t0, t1 = BLK[b]
            n = t1 - t0
            col = 0 if b < 4 else 1
            ksq = 'sqb' + (sfx if sfx_sq is None else sfx_sq)
            krs, ktm = 'rstd' + sfx, 'ntmp' + sfx
            act(sqb[:, :, :n], XT[:, :, t0:t1], AF.Square, XTk(b), [ksq])
            ps, pk = nextps()
            for c in range(8):
                mm(ps[:, :n], ones_bf, sqb[:, c, :n], c == 0, c == 7, [ksq, 'cbf'], [pk])
            act(rstd[:, :n], ps[:, :n], AF.Ln, [pk], [krs], scale=1.0 / D, bias=EPS)
            act(rstd[:, :n], rstd[:, :n], AF.Exp, [krs], [krs], scale=-0.5)
            tt('dve', tmp[:, :, :n], XT[:, :, t0:t1], rstd[:, :n].unsqueeze(1).to_broadcast([128, 8, n]), ALU.mult,
               XTk(b) + [krs], [ktm])
            for c in range(8):
                act(tmp[:, c, :n], tmp[:, c, :n], AF.Identity, [ktm, 'modT', Asc[1]], [ktm],
                    scale=Asc[0][:, c, col:col + 1], bias=modv(mshift, c, col))

        with contextlib.ExitStack() as st2:
            tmp2 = [st2.enter_context(nc.sbuf_tensor("ntmp%d_%d" % (i, l), [128, 8, 512], F32)) for i in range(2)]
            sqb2 = [st2.enter_context(nc.sbuf_tensor("sqb%d_%d" % (i, l), [128, 8, 512], BF16)) for i in range(2)]
            rstd2 = [st2.enter_context(nc.sbuf_tensor("rstd%d_%d" % (i, l), [128, 512], F32)) for i in range(2)]
            hb_t = [st2.enter_context(nc.sbuf_tensor("hbn%d_%d" % (i, l), [128, 8, 512], BF16)) for i in range(2)]
            for b in range(5):
                t0, t1 = BLK[b]
                n = t1 - t0
                x_ = b % 2
                norm_block(b, (A1, 'A1'), 0, tmp2[x_], sqb2[x_], rstd2[x_], str(x_))
                hb = hb_t[x_]
                cp('pool', hb[:, :, :n], tmp2[x_][:, :, :n], ['ntmp%d' % x_], ['hb%d' % x_])
                dma('sp', hT_d[b].rearrange("p (c t) -> p c t", t=512)[:, :, :n], hb[:, :, :n], ['hb%d' % x_], [('hT', b)], 'hs')
        S.barrier()

        stage(1)

        def load_h(b):
            t0, t1 = BLK[b]
            i = ring('hb', len(U['hb']))
            dma('sp', U['hb'][i][:, :, :t1 - t0], hT_d[b].rearrange("p (c t) -> p c t", t=512)[:, :, :t1 - t0], [('hT', b)], ['hb%d' % i], 'hl%d' % i)
            return U['hb'][i], 'hb%d' % i

        pend_h = {}

        def get_h(b):
            if b in pend_h:
                return pend_h.pop(b)
            return load_h(b)

        def prefetch_h(b):
            if b not in pend_h:
                pend_h[b] = load_h(b)

        win = w_in_d[l].rearrange("(kc p) n -> p kc n", p=128)

        def load_w(dst0, src0, ncol, first):
            dma('pool', wph[:, :, dst0:dst0 + ncol], win[:, :, src0:src0 + ncol], [], ['wph'], 'wph')

        def proj_fm(wcol, b, hb, hk):
            t0, t1 = BLK[b]
            n = t1 - t0
            ps, pk = nextps()
            for kc in range(8):
                mm(ps[:, :n], wph[:, kc, wcol:wcol + 128], hb[:, kc, :n], kc == 0, kc == 7, ['wph', hk], [pk])
            return ps, pk

        def proj_tm(wcol, ncol, hb, hk, tl):
            ps, pk = nextps()
            for kc in range(8):
                mm(ps[:, :ncol], hb[:, kc, tl * 128:(tl + 1) * 128], wph[:, kc, wcol:wcol + ncol], kc == 0, kc == 7, ['wph', hk], [pk])
            return ps, pk

        def head_norm(ps, pk, n, gcol, sqh, rs, qn):
            act(sqh[:, :n], ps[:, :n], AF.Square, [pk], ['sqh'])
            p2, p2k = nextps()
            mm(p2[:, :n], onesbd_bf, sqh[:, :n], True, True, ['sqh', 'cbf'], [p2k])
            act(rs[:, :n], p2[:, :n], AF.Ln, [p2k], ['rs'], scale=1.0 / 64, bias=EPS)
            act(rs[:, :n], rs[:, :n], AF.Exp, ['rs'], ['rs'], scale=-0.5)
            stt('dve', qn[:, :n], ps[:, :n], vecs[:, gcol:gcol + 1], rs[:, :n], ALU.mult, ALU.mult, [pk, 'rs', 'vecs'], ['qn'])

        def wout_load(nch, row0):
            wov_ = w_out_d[l].rearrange("(kc p) n -> p kc n", p=128)
            dma('pool', U['woutu'][:, 0:nch, :], wov_[:, row0:row0 + nch, :], [], ['woutu'], 'wo')

        def wout_partial(nch, row0, blocks):
            wov = w_out_d[l].rearrange("(kc p) n -> p kc n", p=128)
            woutu, mixu = U['woutu'], U['mixu']
            for b in blocks:
                t0, t1 = BLK[b]
                n = t1 - t0
                col = 0 if b < 4 else 1
                for dc in range(8):
                    ps, pk = nextps()
                    for kc in range(nch):
                        mm(ps[:, :n], woutu[:, kc, dc * 128:(dc + 1) * 128], mixu[:, kc, t0:t1], kc == 0, kc == nch - 1,
                           ['woutu', ('mixu', b)], [pk])
                    stt('dve', XT[:, dc, t0:t1], ps[:, :n], modv(2, dc, col), XT[:, dc, t0:t1], ALU.mult, ALU.add,
                        [pk, 'modT'] + XTk(b), XTk(b))

        for pr in range(2):
            with contextlib.ExitStack() as st2:
                def sb2(name, shape, dt=F32):
                    return st2.enter_context(nc.sbuf_tensor(name + "_a%d%d" % (l, pr), list(shape), dt))
                unit_bufs(st2, "a%d%d" % (l, pr), 2, 1)
                mixu = U['mixu']
                aq = sb2("aq", [128, T], BF16)
                akz = [sb2("akz%d" % i, [128, T], BF16) for i in range(2)]
                avp = sb2("avp", [128, 18, 2, 128], BF16)
                nabm = sb2("nabm", [128, 2, 15, 64])
                PTa = [sb2("PTa%d" % i, [128, 7, 256], BF16) for i in range(2)]
                sqh = sb2("sqh", [128, 512], BF16)
                rs = sb2("rs", [128, 512])
                qn = sb2("qn", [128, 512])
                rden = [sb2("rden%d" % i, [128, 128]) for i in range(2)]
                load_w(0, pr * 128, 128, True)
                load_w(128, 256 + pr * 128, 128, False)
                load_w(256, 512 + pr * 128, 128, False)
                wout_load(1, pr)
                dma('sp', nabm[:].rearrange("p a b c -> p (a b c)"), nab_d[l][:, pr * 1920:(pr + 1) * 1920], [], ['nabm'], 'c3')
                tt('dve', nabm[:].rearrange("p a b c -> p (a b) c"), nabm[:].rearrange("p a b c -> p (a b) c"),
                   colmask.unsqueeze(1).to_broadcast([128, 30, 64]), ALU.add, ['nabm', 'cf32'], ['nabm'])
                memset('pool', avp[:], 0.0, ['avp'])
                for i in range(2):
                    memset('pool', akz[i][:], 0.0, [('ak', b) for b in range(5)])
                for b in range(5):
                    t0, t1 = BLK[b]
                    n = t1 - t0
                    hb, hk = get_h(b)
                    if b + 1 < 5:
                        prefetch_h(b + 1)
                    if not (b == 4 and last):
                        ps, pk = proj_fm(0, b, hb, hk)
                        head_norm(ps, pk, n, 16, sqh, rs, qn)
                        cp('act', aq[:, t0:t1], qn[:, :n], ['qn'], [('aq', b)])
                    ps, pk = proj_fm(128, b, hb, hk)
                    head_norm(ps, pk, n, 17, sqh, rs, qn)
                    cp('act', akz[0][0:64, t0:t1], qn[0:64, :n], ['qn'], [('ak', b)])
                    cp('act', akz[1][64:128, t0:t1], qn[64:128, :n], ['qn'], [('ak', b)])
                    for tl in range(n // 128):
                        tg = t0 // 128 + tl
                        ps, pk = proj_tm(256, 128, hb, hk, tl)
                        cp('act', avp[:, tg, 0, 0:64], ps[:, 0:64], [pk], ['avp'])
                        cp('dve', avp[:, tg, 1, 64:128], ps[:, 64:128], [pk], ['avp'])
                stage(20)
                qtiles = list(range(16)) + ([] if last else [16, 17])
                na_kts = {}
                cnts = {'s': 0, 'b': 0}

                def na_subs(qt, kt):
                    subs = []
                    for kh in range(2):
                        for qh in range(2):
                            kr = 2 * kt + kh
                            qr = 2 * qt + qh
                            rs0 = min(max(qr - 4, 0), 24)
                            if rs0 <= kr < rs0 + 8:
                                subs.append((kh, qh, kr - qr + 7))
                    return tuple(subs)

                classes = {}
                for qt_ in range(16):
                    for kt_ in range(16):
                        sb_ = na_subs(qt_, kt_)
                        if sb_ and sb_ not in classes:
                            classes[sb_] = len(classes)
                ncls = len(classes)
                EN = sb2("EN", [128, 2, 15, 64], BF16)
                EB = sb2("EB", [128, ncls, 256], BF16)
                PTr = [sb2("PTr%d" % i, [128, 256], BF16) for i in range(3)]
                act(EN[:].rearrange("p a b c -> p (a b c)"), nabm[:].rearrange("p a b c -> p (a b c)"), AF.Exp, ['nabm'], ['EN'])
                memset('pool', EB[:], 0.0, ['EB'])
                for sb_, c_ in classes.items():
                    for (kh, qh, dri) in sb_:
                        o_ap = EB[kh * 64:(kh + 1) * 64, c_, :].rearrange("p (h q) -> p h q", h=2)[:, :, qh * 64:(qh + 1) * 64]
                        cp('dve', o_ap, EN[kh * 64:(kh + 1) * 64, :, dri, :], ['EN'], ['EB'])

                def na_A(qi):
                    qt = qtiles[qi]
                    qb = qt // 4 if qt < 16 else 4
                    kts = []
                    if qt < 16:
                        for kt in range(16):
                            subs = na_subs(qt, kt)
                            if subs:
                                kts.append((kt, subs))
                    kts += [(16, None), (17, None)]
                    assert len(kts) <= 7
                    na_kts[qi] = kts
                    pt = PTa[qi % 2]
                    for j, (kt, subs) in enumerate(kts):
                        kb = kt // 4 if kt < 16 else 4
                        bi = cnts['s'] % 4
                        cnts['s'] += 1
                        ps, pk = psf[bi], 'psf%d' % bi
                        for hh in range(2):
                            mm(ps[:, hh * 128:(hh + 1) * 128], akz[hh][:, kt * 128:(kt + 1) * 128],
                               aq[:, qt * 128:(qt + 1) * 128], True, True, [('ak', kb), ('aq', qb)], [pk])
                        if subs is None:
                            act(pt[:, j, :], ps[:, 0:256], AF.Exp, [pk], [('PTa', qi % 2, j)], scale=0.125)
                        else:
                            si = cnts['b'] % 3
                            cnts['b'] += 1
                            act(PTr[si][:], ps[:, 0:256], AF.Exp, [pk], ['PTr%d' % si], scale=0.125)
                            tt('dve', pt[:, j, :], PTr[si][:], EB[:, classes[subs], :], ALU.mult,
                               ['PTr%d' % si, 'EB'], [('PTa', qi % 2, j)])

                def na_B(qi):
                    qt = qtiles[qi]
                    qb = qt // 4 if qt < 16 else 4
                    kts = na_kts[qi]
                    pt = PTa[qi % 2]
                    nk = len(kts)
                    bo = 4 + 2 * (qi % 2)
                    pO, pD = psf[bo], psf[bo + 1]
                    kO, kD = 'psf%d' % bo, 'psf%d' % (bo + 1)
                    cnt = 0
                    for hh in range(2):
                        for j, (kt, subs) in enumerate(kts):
                            mm(pO[:, 0:128], avp[:, kt, hh, :], pt[:, j, hh * 128:(hh + 1) * 128], cnt == 0, cnt == 2 * nk - 1,
                               ['avp', ('PTa', qi % 2, j)], [kO])
                            cnt += 1
                    cnt = 0
                    for hh in range(2):
                        for j, (kt, subs) in enumerate(kts):
                            mm(pD[:, 0:128], oneshalf[hh], pt[:, j, hh * 128:(hh + 1) * 128], cnt == 0, cnt == 2 * nk - 1,
                               ['cbf', ('PTa', qi % 2, j)], [kD])
                            cnt += 1
                    ri = qi % 2
                    recip(rden[ri][:], pD[:, 0:128], [kD], ['rden%d' % ri])
                    tt('dve', mixu[:, 0, qt * 128:(qt + 1) * 128], pO[:, 0:128], rden[ri][:], ALU.mult, [kO, 'rden%d' % ri], [('mixu', qb)])

                nq = len(qtiles)
                for t_ in range(nq + 1):
                    if t_ < nq:
                        na_A(t_)
                    if t_ >= 1:
                        na_B(t_ - 1)
                if 'mixA%d' % pr in dbg and l == 0:
                    dump('mixA%d' % pr, mixu[:, 0, :], [('mixu', b) for b in range(5)], [128, T], BF16)
                wout_partial(1, pr, range(nblk))
            S.barrier()

        stage(2)
        for g in range(2):
            with contextlib.ExitStack() as st2:
                def sb2(name, shape, dt=F32):
                    return st2.enter_context(nc.sbuf_tensor(name + "_b%d%d" % (l, g), list(shape), dt))
                unit_bufs(st2, "b%d%d" % (l, g), 2, 2)
                mixu = U['mixu']
                bq = sb2("bq", [128, 2, T], BF16)
                kkz = [sb2("kkz%d" % i, [128, T], BF16) for i in range(2)]
                vpd = sb2("vpd", [128, 18, 2, 128], BF16)
                ropet = sb2("ropet", [128, 2, SL])
                PT = [sb2("PT%d" % i, [128, 512], BF16) for i in range(4)]
                sqh = sb2("sqh", [128, 512], BF16)
                rs = sb2("rs", [128, 512])
                qn = sb2("qn", [128, 512])
                qnb = sb2("qnb", [128, 512], BF16)
                r1 = sb2("r1", [128, 512])
                r2 = sb2("r2", [128, 512])
                rden = [sb2("rden%d" % i, [128, 512]) for i in range(2)]
                dma('sp', ropet[:], rope_d, [], ['ropet'], 'c3')
                load_w(0, 768 + g * 256, 256, True)
                load_w(256, 1280 + g * 64, 64, False)
                load_w(320, 1280 + g * 64, 64, False)
                load_w(384, 1408 + g * 64, 64, False)
                wout_load(2, 2 + 2 * g)
                memset('pool', vpd[:], 0.0, ['vpd'])
                for i in range(2):
                    memset('pool', kkz[i][:], 0.0, [('kk', b) for b in range(5)])
                for b in range(5):
                    t0, t1 = BLK[b]
                    n = t1 - t0
                    hb, hk = get_h(b)
                    if b + 1 < 5:
                        prefetch_h(b + 1)
                    for ch in range(3):
                        if ch < 2 and b == 4 and last:
                            continue
                        ps, pk = proj_fm(ch * 128, b, hb, hk)
                        head_norm(ps, pk, n, 18 if ch < 2 else 19, sqh, rs, qn)
                        dk = ('bq', ch, b) if ch < 2 else ('kk', b)
                        if ch < 2:
                            dsts = [(bq[:, ch, t0:t1], slice(0, 128))]
                        else:
                            dsts = [(kkz[0][0:64, t0:t1], slice(0, 64)), (kkz[1][64:128, t0:t1], slice(64, 128))]
                        if b == 4:
                            for dst, sl in dsts:
                                cp('act', dst, qn[sl, :n], ['qn'], [dk])
                        else:
                            cp('act', qnb[:, :n], qn[:, :n], ['qn'], ['qnb'])
                            p2, p2k = nextps()
                            mm(p2[:, :n], swap_bf, qnb[:, :n], True, True, ['qnb', 'cbf'], [p2k])
                            tt('pool', r1[:, :n], qn[:, :n], ropet[:, 0, t0:t1], ALU.mult, ['qn', 'ropet'], ['r1'])
                            tt('dve', r2[:, :n], p2[:, :n], ropet[:, 1, t0:t1], ALU.mult, [p2k, 'ropet'], ['r2'])
                            for dst, sl in dsts:
                                tt('dve', dst, r1[sl, :n], r2[sl, :n], ALU.add, ['r1', 'r2'], [dk])
                    for tl in range(n // 128):
                        tg = t0 // 128 + tl
                        ps, pk = proj_tm(384, 64, hb, hk, tl)
                        cp('act', vpd[:, tg, 0, 0:64], ps[:, 0:64], [pk], ['vpd'])
                        cp('dve', vpd[:, tg, 1, 64:128], ps[:, 0:64], [pk], ['vpd'])
                items = []
                gi = 0
                for qb in range(nblk):
                    kts = list(range(18)) if qb < 4 else [16, 17]
                    for ch in range(2):
                        lst = [(hh, kt) for hh in range(2) for kt in kts]
                        for j, (hh, kt) in enumerate(lst):
                            items.append((gi, qb, ch, hh, kt, j == 0, j == len(lst) - 1))
                        gi += 1

                def gq_A(i):
                    gi, qb, ch, hh, kt, first, lastk = items[i]
                    t0, t1 = BLK[qb]
                    n = t1 - t0
                    kb = kt // 4 if kt < 16 else 4
                    bi = i % 4
                    ps, pk = psf[bi], 'psf%d' % bi
                    mm(ps[:, :n], kkz[hh][:, kt * 128:(kt + 1) * 128], bq[:, ch, t0:t1],
                       True, True, [('kk', kb), ('bq', ch, qb)], [pk])
                    act(PT[bi][:, :n], ps[:, :n], AF.Exp, [pk], ['PT%d' % bi], scale=0.125)

                def gq_B(i):
                    gi, qb, ch, hh, kt, first, lastk = items[i]
                    t0, t1 = BLK[qb]
                    n = t1 - t0
                    bi = i % 4
                    bo = 4 + 2 * (gi % 2)
                    pO, pD = psf[bo], psf[bo + 1]
                    kO, kD = 'psf%d' % bo, 'psf%d' % (bo + 1)
                    mm(pO[:, :n], vpd[:, kt, hh, :], PT[bi][:, :n], first, lastk, ['vpd', 'PT%d' % bi], [kO])
                    mm(pD[:, :n], oneshalf[hh], PT[bi][:, :n], first, lastk, ['cbf', 'PT%d' % bi], [kD])
                    if lastk:
                        ri = gi % 2
                        recip(rden[ri][:, :n], pD[:, :n], [kD], ['rden%d' % ri])
                        tt('dve', mixu[:, ch, t0:t1], pO[:, :n], rden[ri][:, :n], ALU.mult, [kO, 'rden%d' % ri], [('mixu', qb)])

                ni = len(items)
                for t_ in range(ni + 2):
                    if t_ < ni:
                        gq_A(t_)
                    if t_ >= 2:
                        gq_B(t_ - 2)
                if 'mixB%d' % g in dbg and l == 0:
                    dump('mixB%d' % g, mixu[:, :, :], [('mixu', b) for b in range(5)], [128, 2, T], BF16)
                wout_partial(2, 2 + 2 * g, range(nblk))
            S.barrier()

        stage(3)
        for pr in range(2):
            with contextlib.ExitStack() as st2:
                def sb2(name, shape, dt=F32):
                    return st2.enter_context(nc.sbuf_tensor(name + "_c%d%d" % (l, pr), list(shape), dt))
                qs = sb2("qs", [128, T], BF16)
                unit_bufs(st2, "c%d%d" % (l, pr), 1, 1)
                mixu = U['mixu']
                vpc = sb2("vpc", [128, 18, 2, 128], BF16)
                sgate = sb2("sgate", [128, T], BF16)
                osum = sb2("osum", [128, T])
                t1b = sb2("t1b", [128, 512])
                t2b = sb2("t2b", [128, 512])
                t3b = sb2("t3b", [128, 512])
                t4b = sb2("t4b", [128, 512])
                t5b = sb2("t5b", [128, 512])
                ablk = [sb2("ablk%d" % i, [128, 32]) for i in range(2)]
                qh = [sb2("qh%d" % i, [128, 512], BF16) for i in range(2)]
                kcbz = [[sb2("kcbz%d%d" % (i, j), [128, 512], BF16) for j in range(2)] for i in range(2)]
                kbb = [sb2("kbb%d" % i, [128, 512], BF16) for i in range(2)]
                kbT = [[sb2("kbT%d%d" % (i, j), [128, 128], BF16) for j in range(2)] for i in range(2)]
                vblk = [[sb2("vblk%d%d" % (i, j), [128, 8, 128], BF16) for j in range(2)] for i in range(2)]
                U9 = [sb2("U9%d" % i, [128, 128, 9]) for i in range(3)]
                A9 = [sb2("A9%d" % i, [128, 128, 9], BF16) for i in range(2)]
                S9 = [sb2("S9%d" % i, [128, 128, 9], BF16) for i in range(2)]
                PA = [[sb2("PA%d%d" % (i, j), [128, 128], BF16) for j in range(2)] for i in range(3)]
                oi = [sb2("oi%d" % i, [128, 128]) for i in range(2)]
                sqh = qh[0]
                rs = t1b
                load_w(0, 1536 + pr * 128, 128, True)
                load_w(128, 2048 + pr * 128, 128, False)
                load_w(256, 2304 + pr * 128, 128, False)
                load_w(384, 2560 + pr * 128, 128, False)
                load_w(512, 1792 + pr * 128, 128, False)
                wout_load(1, 6 + pr)
                memset('pool', vpc[:], 0.0, ['vpc'])
                for i in range(2):
                    for j in range(2):
                        memset('pool', kcbz[i][j][:], 0.0, ['kcb%d' % i])
                for i in range(2):
                    memset('pool', A9[i][:], 0.0, ['A9%d' % i])
                for i in range(2):
                    for j in range(2):
                        memset('pool', kbT[i][j][:], 0.0, ['kbT%d' % i])
                        memset('pool', vblk[i][j][:], 0.0, ['vblk%d' % i])
                for b in range(5):
                    t0, t1 = BLK[b]
                    n = t1 - t0
                    hb, hk = get_h(b)
                    ps, pk = proj_fm(0, b, hb, hk)
                    act(qs[:, t0:t1], ps[:, :n], AF.Copy, [pk], [('qs', b)], scale=0.125)
                    if not (b == 4 and last):
                        ps, pk = proj_fm(384, b, hb, hk)
                        act(t1b[:, :n], ps[:, :n], AF.Exp, [pk], ['t1b'], scale=-1.0)
                        act(t1b[:, :n], t1b[:, :n], AF.Ln, ['t1b'], ['t1b'], bias=1.0)
                        act(t1b[:, :n], t1b[:, :n], AF.Exp, ['t1b'], ['t1b'], scale=-1.0)
                        tt('dve', sgate[:, t0:t1], t1b[:, :n], ps[:, :n], ALU.mult, ['t1b', pk], [('sgate', b)])
                    for tl in range(n // 128):
                        tg = t0 // 128 + tl
                        ps, pk = proj_tm(512, 128, hb, hk, tl)
                        cp('dve', vpc[:, tg, 0, 0:64], ps[:, 0:64], [pk], ['vpc'])
                        cp('dve', vpc[:, tg, 1, 64:128], ps[:, 64:128], [pk], ['vpc'])
                    if b + 1 < 5:
                        prefetch_h(b + 1)
                    else:
                        pend_h[4] = (hb, hk)
                for dr in range(2):
                    oml = lbv[:, dr * 2 + pr:dr * 2 + pr + 1]
                    lbf = lbv[:, 4 + dr * 2 + pr:4 + dr * 2 + pr + 1]
                    border = [4, 0, 1, 2, 3] if dr == 0 else [4, 3, 2, 1, 0]
                    tiles = []
                    for bi, b in enumerate(border):
                        ntl = (BLK[b][1] - BLK[b][0]) // 128
                        tls = list(range(ntl)) if dr == 0 else list(range(ntl - 1, -1, -1))
                        for k_, tl in enumerate(tls):
                            tiles.append((bi, b, tl, k_ == 0))

                    def stage0_ops(bi, b):
                        bs = bi % 2
                        t0, t1 = BLK[b]
                        n = t1 - t0
                        nch = n // 16
                        ops_ = []
                        cell = {}

                        def o1():
                            hb, hk = get_h(b)
                            cell['ps'], cell['pk'] = proj_fm(128 + 128 * dr, b, hb, hk)
                            if bi + 1 < len(border):
                                prefetch_h(border[bi + 1])
                            elif dr == 0:
                                pend_h[4] = (hb, hk) if b == 4 else load_h(4)
                            act(t1b[:, :n], cell['ps'][:, :n], AF.Exp, [cell['pk']], ['t1b'], scale=-1.0)
                        ops_.append(o1)
                        ops_.append(lambda: act(t1b[:, :n], t1b[:, :n], AF.Ln, ['t1b'], ['t1b'], bias=1.0))
                        ops_.append(lambda: act(t1b[:, :n], t1b[:, :n], AF.Exp, ['t1b'], ['t1b'], scale=-1.0))
                        ops_.append(lambda: ts('dve', t1b[:, :n], t1b[:, :n], oml, lbf, ALU.mult, ALU.add, ['t1b', 'lbv'], ['t1b']))
                        ops_.append(lambda: ts('pool', t2b[:, :n], t1b[:, :n], -1.0, 1.0, ALU.mult, ALU.add, ['t1b'], ['t2b']))
                        ops_.append(lambda: act(t1b[:, :n], t1b[:, :n], AF.Ln, ['t1b'], ['t1b']))
                        ops_.append(lambda: S.op('dve', lambda e: e.tensor_tensor_scan(out=t3b[:, :n], data0=scanmask[:, :n], data1=t1b[:, :n],
                                                                                       initial=0.0, op0=ALU.mult, op1=ALU.add),
                                                 r=['t1b', 'cf32'], w=['t3b']))
                        c3v = t3b[:, :n].rearrange("p (c s) -> p c s", s=16)
                        if dr == 0:
                            cd, cdk = t3b, 't3b'
                            cend = c3v[:, :, 15:16]
                        else:
                            ops_.append(lambda: tt('dve', t4b[:, :n].rearrange("p (c s) -> p c s", s=16), c3v[:, :, 15:16].to_broadcast([128, nch, 16]),
                                                   c3v, ALU.subtract, ['t3b'], ['t4b']))
                            ops_.append(lambda: tt('dve', t4b[:, :n], t4b[:, :n], t1b[:, :n], ALU.add, ['t4b', 't1b'], ['t4b']))
                            cd, cdk = t4b, 't4b'
                            cend = t4b[:, :n].rearrange("p (c s) -> p c s", s=16)[:, :, 0:1]
                        ops_.append(lambda: act(ablk[bs][:, :nch].unsqueeze(2), cend, AF.Exp, [cdk], ['ablk%d' % bs]))
                        ops_.append(lambda: act(t5b[:, :n], cd[:, :n], AF.Exp, [cdk], ['t5b']))
                        ops_.append(lambda: tt('dve', qh[bs][:, :n], qs[:, t0:t1], t5b[:, :n], ALU.mult, [('qs', b), 't5b'], ['qh%d' % bs]))
                        ops_.append(lambda: act(t5b[:, :n], cd[:, :n], AF.Exp, [cdk], ['t5b'], scale=-1.0))
                        ops_.append(lambda: tt('dve', kcbz[bs][0][0:64, :n], t2b[0:64, :n], t5b[0:64, :n], ALU.mult, ['t2b', 't5b'], ['kcb%d' % bs]))
                        ops_.append(lambda: tt('dve', kcbz[bs][1][64:128, :n], t2b[64:128, :n], t5b[64:128, :n], ALU.mult, ['t2b', 't5b'], ['kcb%d' % bs]))
                        ops_.append(lambda: tt('dve', t5b[:, :n].rearrange("p (c s) -> p c s", s=16), cend.to_broadcast([128, nch, 16]),
                                               cd[:, :n].rearrange("p (c s) -> p c s", s=16), ALU.subtract, [cdk], ['t5b']))
                        ops_.append(lambda: act(t5b[:, :n], t5b[:, :n], AF.Exp, ['t5b'], ['t5b']))
                        ops_.append(lambda: tt('dve', kbb[bs][:, :n], t2b[:, :n], t5b[:, :n], ALU.mult, ['t2b', 't5b'], ['kbb%d' % bs]))
                        return ops_

                    pend = {'ops': [], 'per': 0}

                    def drain(k):
                        for _ in range(k):
                            if pend['ops']:
                                pend['ops'].pop(0)()

                    def hg_A(ti):
                        bi, b, tl, firstb = tiles[ti]
                        if firstb:
                            for f_ in stage0_ops(bi, b):
                                f_()
                        bs = bi % 2
                        x = ti % 2
                        x3 = ti % 3
                        t0 = BLK[b][0]
                        tg = t0 // 128 + tl
                        c0 = tl * 128
                        S.op('pe', lambda e, x=x, c0=c0, bs=bs: e.transpose(psb[x][:, 0:128], kbb[bs][:, c0:c0 + 128], ident_bf),
                             r=['kbb%d' % bs, 'cbf'], w=['psf%d' % (6 + x)])
                        cp('act', kbT[x][0][:, 0:64], psb[x][:, 0:64], ['psf%d' % (6 + x)], ['kbT%d' % x])
                        cp('act', kbT[x][1][:, 64:128], psb[x][:, 64:128], ['psf%d' % (6 + x)], ['kbT%d' % x])
                        cmv = cmfull.rearrange("p (a b) -> p a b", b=128)
                        for hh in range(2):
                            tt('pool', vblk[x][hh][:, :, hh * 64:(hh + 1) * 64],
                               vpc[:, tg, hh, hh * 64:(hh + 1) * 64].unsqueeze(1).to_broadcast([128, 8, 64]),
                               cmv[:, :, hh * 64:(hh + 1) * 64], ALU.mult, ['vpc', 'cf32'], ['vblk%d' % x])
                        for half in range(2):
                            pu = psf[2 + half]
                            for hh in range(2):
                                mm(pu[:, :], kbT[x][hh][:], vblk[x][hh][:, half * 4:(half + 1) * 4, :].rearrange("p a b -> p (a b)"),
                                   hh == 0, hh == 1, ['kbT%d' % x, 'vblk%d' % x], ['psf%d' % (2 + half)])
                            if dr == 0:
                                o_ap = U9[x3][:, :, 1 + half * 4:1 + half * 4 + 4].rearrange("p v j -> p j v")
                            else:
                                base = U9[x3][:, :, 8 - 4 * half:9 - 4 * half]
                                o_ap = bass.AP(base.tensor, base.offset, [list(base.ap[0]), [-1, 4], [9, 128]])
                            cp('act' if half == 0 else 'dve', o_ap, pu[:, :].rearrange("p (a b) -> p a b", b=128),
                               ['psf%d' % (2 + half)], ['U9%d' % x3])
                        for hh in range(2):
                            ps, pk = psf[hh], 'psf%d' % hh
                            mm(ps[:, 0:128], kcbz[bs][hh][:, c0:c0 + 128], qh[bs][:, c0:c0 + 128],
                               True, True, ['kcb%d' % bs, 'qh%d' % bs], [pk])
                            tt('dve', PA[x3][hh][:], ps[:, 0:128], Mdir[dr], ALU.mult, [pk, 'cf32'], ['PA%d%d' % (x3, hh)])

                    def hg_carry(ti):
                        bi, b, tl, firstb = tiles[ti]
                        bs = bi % 2
                        x = ti % 2
                        x3 = ti % 3
                        abase = ablk[bs][:, tl * 8 + (0 if dr == 0 else 7):tl * 8 + (0 if dr == 0 else 7) + 1]
                        a_in = bass.AP(abase.tensor, abase.offset, [list(abase.ap[0]), [0, 128], [1 if dr == 0 else -1, 8]])
                        act(A9[x][:, :, 1:9], a_in, AF.Copy, ['ablk%d' % bs], ['A9%d' % x])
                        if ti == 0:
                            memset('pool', U9[x3][:, :, 0:1], 0.0, ['U9%d' % x3])
                        else:
                            cp('act', U9[x3][:, :, 0:1], S9[1 - x][:, :, 8:9], ['S9%d' % (1 - x)], ['U9%d' % x3])

                    def hg_scan(ti):
                        x = ti % 2
                        x3 = ti % 3
                        S.op('dve', lambda e, x=x, x3=x3: e.tensor_tensor_scan(out=S9[x][:].rearrange("p v j -> p (v j)"),
                                                                               data0=A9[x][:].rearrange("p v j -> p (v j)"),
                                                                               data1=U9[x3][:].rearrange("p v j -> p (v j)"),
                                                                               initial=0.0, op0=ALU.mult, op1=ALU.add),
                             r=['A9%d' % x, 'U9%d' % x3], w=['S9%d' % x])

                    def hg_out(ti):
                        bi, b, tl, firstb = tiles[ti]
                        bs = bi % 2
                        x = ti % 2
                        x3 = ti % 3
                        t0 = BLK[b][0]
                        tg = t0 // 128 + tl
                        c0 = tl * 128
                        order = list(range(8)) if dr == 0 else list(range(7, -1, -1))
                        pI = psf[4]
                        for j, ci in enumerate(order):
                            mm(pI[:, ci * 16:(ci + 1) * 16], S9[x][:, :, j], qh[bs][:, c0 + ci * 16:c0 + (ci + 1) * 16], True, True,
                               ['S9%d' % x, 'qh%d' % bs], ['psf4'])
                        cp('act', oi[x][:], pI[:, 0:128], ['psf4'], ['oi%d' % x])
                        pOh = psf[5]
                        for hh in range(2):
                            mm(pOh[:, 0:128], vpc[:, tg, hh, :], PA[x3][hh][:], hh == 0, hh == 1, ['vpc', 'PA%d%d' % (x3, hh)], ['psf5'])
                        osl = osum[:, t0 + c0:t0 + c0 + 128]
                        if dr == 0:
                            tt('dve', osl, pOh[:, 0:128], oi[x][:], ALU.add, ['psf5', 'oi%d' % x], [('osum', b)])
                        else:
                            tt('dve', oi[x][:], pOh[:, 0:128], oi[x][:], ALU.add, ['psf5', 'oi%d' % x], ['oi%d' % x])
                            tt('pool', osl, osl, oi[x][:], ALU.add, ['oi%d' % x, ('osum', b)], [('osum', b)])

                    ntile = len(tiles)
                    for t_ in range(-1, ntile + 1):
                        if 0 <= t_ - 1 < ntile:
                            hg_scan(t_ - 1)
                        if 0 <= t_ + 1 < ntile:
                            hg_A(t_ + 1)
                        if 0 <= t_ < ntile:
                            hg_carry(t_)
                        if 0 <= t_ - 1 < ntile:
                            hg_out(t_ - 1)
                for b in range(nblk):
                    t0, t1 = BLK[b]
                    n = t1 - t0
                    act(sqh[:, :n], osum[:, t0:t1], AF.Square, [('osum', b)], ['qh0'])
                    p2, p2k = nextps()
                    mm(p2[:, :n], onesbd_bf, sqh[:, :n], True, True, ['qh0', 'cbf'], [p2k])
                    act(rs[:, :n], p2[:, :n], AF.Ln, [p2k], ['t1b'], scale=1.0 / 64, bias=EPS)
                    act(rs[:, :n], rs[:, :n], AF.Exp, ['t1b'], ['t1b'], scale=-0.5)
                    stt('dve', rs[:, :n], osum[:, t0:t1], vecs[:, 20:21], rs[:, :n], ALU.mult, ALU.mult, [('osum', b), 't1b', 'vecs'], ['t1b'])
                    tt('dve', mixu[:, 0, t0:t1], rs[:, :n], sgate[:, t0:t1], ALU.mult, ['t1b', ('sgate', b)], [('mixu', b)])
                if 'mixC%d' % pr in dbg and l == 0:
                    dump('mixC%d' % pr, mixu[:, 0, :], [('mixu', b) for b in range(5)], [128, T], BF16)
                    dump('osum%d' % pr, osum[:, :], [('osum', b) for b in range(5)], [128, T], F32)
                wout_partial(1, 6 + pr, range(nblk))
            S.barrier()
        stage(4)
        if 'xmid' in dbg and l == 0:
            dump('xmid', XT[:, :, :], [('XT', b) for b in range(5)], [128, 8, T], F32)
            S.barrier()
        stm.close()

        with contextlib.ExitStack() as st2:
            def sb2(name, shape, dt=F32):
                return st2.enter_context(nc.sbuf_tensor(name + "_m%d" % l, list(shape), dt))
            h2T = sb2("h2T", [128, 8, T], BF16)
            combT = sb2("combT", [32, T])
            wrt = sb2("wrt", [128, 8, 36])
            brt = sb2("brt", [128, 36])
            dma('sp', wrt[:], wr_d[l].rearrange("(kc p) n -> p kc n", p=128), [], ['wrt'], 'c3')
            dma('sp', brt[:], brb_d[l], [], ['brt'], 'c3')
            with contextlib.ExitStack() as st3:
                def sb3(name, shape, dt=F32):
                    return st3.enter_context(nc.sbuf_tensor(name + "_r%d" % l, list(shape), dt))
                tmp2 = [sb3("ntmp%d" % i, [128, 8, 512]) for i in range(2)]
                sqb = sb3("sqb", [128, 8, 512], BF16)
                rstd2 = [sb3("rstd%d" % i, [128, 512]) for i in range(2)]
                lg = sb3("lg", [128, 4, 36])
                gmax = sb3("gmax", [128, 4])
                gsh = sb3("gsh", [128, 4, 4])
                gsum = sb3("gsum", [128, 4])
                ghot = sb3("ghot", [128, 4, 4])
                msk = sb3("msk", [128, 4, 4, 8])
                ing = sb3("ing", [128, 4, 8])
                ing2 = sb3("ing2", [128, 4, 8])
                m1 = sb3("m1", [128, 4])
                m2 = sb3("m2", [128, 4])
                hot1 = sb3("hot1", [128, 4, 8])
                hot2 = sb3("hot2", [128, 4, 8])
                w1 = sb3("w1", [128, 4])
                w2 = sb3("w2", [128, 4])
                ce = sb3("ce", [128, 4, 8])
                comb = sb3("comb", [128, 4, 4, 8])
                norm_block(0, (A2, 'A2'), 3, tmp2[0], sqb, rstd2[0], '0', '')
                for b in range(nblk):
                    t0, t1 = BLK[b]
                    n = t1 - t0
                    nt = n // 128
                    tmp = tmp2[b % 2]
                    ktm = 'ntmp%d' % (b % 2)
                    if b + 1 < nblk:
                        nb_ = (b + 1) % 2
                        norm_block(b + 1, (A2, 'A2'), 3, tmp2[nb_], sqb, rstd2[nb_], str(nb_), '')
                    cp('pool', h2T[:, :, t0:t1], tmp[:, :, :n], [ktm], [('h2T', b)])
                    pR = psf[4]
                    for tl in range(nt):
                        for kc in range(8):
                            mm(pR[:, tl * 36:(tl + 1) * 36], tmp[:, kc, tl * 128:(tl + 1) * 128], wrt[:, kc, :], kc == 0, kc == 7,
                               [ktm, 'wrt'], ['pR'])
                    R = ['rt']
                    tt('dve', lg[:, :nt, :], pR[:, 0:nt * 36].rearrange("p (a b) -> p a b", b=36),
                       brt[:].unsqueeze(1).to_broadcast([128, nt, 36]), ALU.add, ['pR', 'brt'], R)
                    G = lg[:, :nt, 0:4]
                    S.op('dve', lambda e, nt=nt, G=G: e.tensor_reduce(out=gmax[:, :nt], in_=G, axis=AX.X, op=ALU.max), r=R, w=R)
                    tt('dve', gsh[:, :nt, :], G, gmax[:, :nt].unsqueeze(2).to_broadcast([128, nt, 4]), ALU.subtract, R, R)
                    tt('dve', ghot[:, :nt, :], G, gmax[:, :nt].unsqueeze(2).to_broadcast([128, nt, 4]), ALU.is_ge, R, R)
                    act(gsh[:, :nt, :], gsh[:, :nt, :], AF.Exp, R, R)
                    S.op('dve', lambda e, nt=nt: e.tensor_reduce(out=gsum[:, :nt], in_=gsh[:, :nt, :], axis=AX.X, op=ALU.add), r=R, w=R)
                    recip(gsum[:, :nt], gsum[:, :nt], R, R)
                    E4 = lg[:, :nt, 4:36].rearrange("p t (g e) -> p t g e", e=8)
                    tt('dve', msk[:, :nt], E4, ghot[:, :nt, :].unsqueeze(3).to_broadcast([128, nt, 4, 8]), ALU.mult, R, R)
                    S.op('dve', lambda e, nt=nt: e.tensor_reduce(out=ing[:, :nt, :], in_=msk[:, :nt].rearrange("p t g e -> p t e g"),
                                                                 axis=AX.X, op=ALU.add), r=R, w=R)
                    S.op('dve', lambda e, nt=nt: e.tensor_reduce(out=m1[:, :nt], in_=ing[:, :nt, :], axis=AX.X, op=ALU.max), r=R, w=R)
                    tt('dve', hot1[:, :nt, :], ing[:, :nt, :], m1[:, :nt].unsqueeze(2).to_broadcast([128, nt, 8]), ALU.is_ge, R, R)
                    stt('dve', ing2[:, :nt, :], hot1[:, :nt, :], -1e30, ing[:, :nt, :], ALU.mult, ALU.add, R, R)
                    S.op('dve', lambda e, nt=nt: e.tensor_reduce(out=m2[:, :nt], in_=ing2[:, :nt, :], axis=AX.X, op=ALU.max), r=R, w=R)
                    tt('dve', hot2[:, :nt, :], ing2[:, :nt, :], m2[:, :nt].unsqueeze(2).to_broadcast([128, nt, 8]), ALU.is_ge, R, R)
                    tt('dve', w2[:, :nt], m2[:, :nt], m1[:, :nt], ALU.subtract, R, R)
                    act(w2[:, :nt], w2[:, :nt], AF.Exp, R, R)
                    ts('dve', w1[:, :nt], w2[:, :nt], 1.0, None, ALU.add, None, R, R)
                    recip(w1[:, :nt], w1[:, :nt], R, R)
                    tt('dve', w2[:, :nt], w2[:, :nt], w1[:, :nt], ALU.mult, R, R)
                    tt('dve', w1[:, :nt], w1[:, :nt], gsum[:, :nt], ALU.mult, R, R)
                    tt('dve', w2[:, :nt], w2[:, :nt], gsum[:, :nt], ALU.mult, R, R)
                    tt('dve', ce[:, :nt, :], hot1[:, :nt, :], w1[:, :nt].unsqueeze(2).to_broadcast([128, nt, 8]), ALU.mult, R, R)
                    tt('dve', hot2[:, :nt, :], hot2[:, :nt, :], w2[:, :nt].unsqueeze(2).to_broadcast([128, nt, 8]), ALU.mult, R, R)
                    tt('dve', ce[:, :nt, :], ce[:, :nt, :], hot2[:, :nt, :], ALU.add, R, R)
                    tt('dve', comb[:, :nt], ghot[:, :nt, :].unsqueeze(3).to_broadcast([128, nt, 4, 8]),
                       ce[:, :nt, :].unsqueeze(2).to_broadcast([128, nt, 4, 8]), ALU.mult, R, R)
                    pX = psf[5]
                    for tl in range(nt):
                        mm(pX[0:32, tl * 128:(tl + 1) * 128], comb[:, tl].rearrange("p g e -> p (g e)"), ident_f, True, True,
                           R + ['cf32'], ['pX'])
                    cp('act', combT[:, t0:t1], pX[0:32, 0:n], ['pX'], [('combT', b)])
                if 'combT' in dbg and l == 0:
                    dump('combT', combT[:, :], [('combT', b) for b in range(5)], [32, T], F32)
                    dump('h2T', h2T[:, :, :], [('h2T', b) for b in range(5)], [128, 8, T], BF16)
            dma('sp', comb_d[:, 0:BLK[nblk - 1][1]], combT[:, 0:BLK[nblk - 1][1]], [('combT', b) for b in range(nblk)], ['combd'], 'cbd')
            S.barrier()
            stage(5)
            we = [sb2("we%d" % i, [128, 6144], BF16) for i in range(2)]
            hid = [sb2("hid%d" % i, [128, 2, 512], BF16) for i in range(2)]
            cbcE = [sb2("cbcE%d" % i, [128, T]) for i in range(2)]
            sgs = [sb2("sgs%d" % i, [128, 512], BF16) for i in range(4)]
            ut = [sb2("ut%d" % i, [128, 512]) for i in range(4)]
            def load_exp(ex):
                wt = we[ex % 2]
                wk = 'we%d' % (ex % 2)
                dma('pool', wt[:, 0:2048], weg_d[l, ex], [], [wk], wk)
                dma('pool', wt[:, 2048:4096], weu_d[l, ex], [], [wk], wk)
                dma('pool', wt[:, 4096:6144], wed_d[l, ex], [], [wk], wk)

            def load_cbc(ex):
                i_ = ex % 2
                dma('sp', cbcE[i_][:, :], comb_d[ex:ex + 1, :].partition_broadcast(128), ['combd'], ['cbcE%d' % i_], 'cbl%d' % i_)

            its = [(ex, b) for ex in range(32) for b in range(nblk)]

            def moe_A(i):
                ex, b = its[i]
                wt = we[ex % 2]
                wk = 'we%d' % (ex % 2)
                wg = wt[:, 0:2048].rearrange("p (k f) -> p k f", f=256)
                wu = wt[:, 2048:4096].rearrange("p (k f) -> p k f", f=256)
                t0, t1 = BLK[b]
                n = t1 - t0
                i2 = i % 2
                for fc in range(2):
                    pG, pGk = psf[fc], 'psf%d' % fc
                    pU, pUk = psf[2 + fc], 'psf%d' % (2 + fc)
                    for kc in range(8):
                        mm(pG[:, :n], wg[:, kc, fc * 128:(fc + 1) * 128], h2T[:, kc, t0:t1], kc == 0, kc == 7, [wk, ('h2T', b)], [pGk])
                    for kc in range(8):
                        mm(pU[:, :n], wu[:, kc, fc * 128:(fc + 1) * 128], h2T[:, kc, t0:t1], kc == 0, kc == 7, [wk, ('h2T', b)], [pUk])
                    i3 = i2 * 2 + fc
                    act(sgs[i3][:, :n], pG[:, :n], AF.Silu, [pGk], ['sgs%d' % i3])
                    tt('dve', ut[i3][:, :n], pU[:, :n], cbcE[ex % 2][:, t0:t1], ALU.mult, [pUk, 'cbcE%d' % (ex % 2)], ['ut%d' % i3])
                    tt('pool', hid[i2][:, fc, :n], ut[i3][:, :n], sgs[i3][:, :n], ALU.mult, ['ut%d' % i3, 'sgs%d' % i3], ['hid%d' % i2])

            def moe_B(i):
                ex, b = its[i]
                wt = we[ex % 2]
                wk = 'we%d' % (ex % 2)
                wd = wt[:, 4096:6144].rearrange("p (k f) -> p k f", f=1024)
                t0, t1 = BLK[b]
                n = t1 - t0
                col = 0 if b < 4 else 1
                i2 = i % 2
                for dc in range(8):
                    bd_ = 4 + dc % 4
                    pD = psf[bd_]
                    for fc in range(2):
                        mm(pD[:, :n], wd[:, fc, dc * 128:(dc + 1) * 128], hid[i2][:, fc, :n], fc == 0, fc == 1, [wk, 'hid%d' % i2], ['psf%d' % bd_])
                    stt('dve', XT[:, dc, t0:t1], pD[:, :n], modv(5, dc, col), XT[:, dc, t0:t1], ALU.mult, ALU.add,
                        ['psf%d' % bd_, 'modT'] + XTk(b), XTk(b))

            load_exp(0)
            load_exp(1)
            load_cbc(0)
            load_cbc(1)
            nit = len(its)
            for t_ in range(nit + 1):
                if t_ < nit:
                    moe_A(t_)
                if t_ >= 1:
                    moe_B(t_ - 1)
                if t_ < nit and its[t_][1] == 0 and 1 <= its[t_][0] < 31:
                    load_exp(its[t_][0] + 1)
                    load_cbc(its[t_][0] + 1)
        S.barrier()
        if 'xl0' in dbg and l == 0:
            dump('xl0', XT[:, :, :], [('XT', b) for b in range(5)], [128, 8, T], F32)

    try:
        for l in range(n_layers):
            layer(l)
    except _Stop:
        stopped = True
    else:
        stopped = False
    ov = outT.rearrange("(c p) t -> p c t", p=128)
    for c in range(8):
        dma('sp', ov[:, c, :], XT[:, c, 0:SL], [('XT', b) for b in range(4)], ['out'], 'o%d' % (c % 4))
    S.op('sp', None, r=['out'] + ['dbg_' + k for k in dbg_out])
    S.emit()
    if not stopped:
        st.close()
    return nc, dbg_out


def _consts():
    p = np.arange(128)
    ident = np.eye(128, dtype=np.float32)
    swap = np.zeros((128, 128), np.float32)
    swap[p, p ^ 1] = 1.0
    onesbd = (p[:, None] // 64 == p[None, :] // 64).astype(np.float32)
    ones = np.ones((128, 128), np.float32)
    oh0 = np.broadcast_to((p[None, :] // 64 == 0), (128, 128)).astype(np.float32)
    oh1 = np.broadcast_to((p[None, :] // 64 == 1), (128, 128)).astype(np.float32)
    cbf = np.concatenate([ident, swap, onesbd, ones, oh0, oh1], axis=1)
    ck = p % 64
    cq = np.arange(64)
    cs = np.clip(cq - 8, 0, 48)
    ok = (ck[:, None] >= cs[None, :]) & (ck[:, None] < cs[None, :] + 16)
    colmask = np.where(ok, 0.0, -1e30).astype(np.float32)
    same = (p[:, None] // 16 == p[None, :] // 16)
    Mf = (same & (p[:, None] <= p[None, :])).astype(np.float32)
    Mb = (same & (p[:, None] >= p[None, :])).astype(np.float32)
    bd = onesbd
    cm = (p[:, None] // 16 == np.arange(8)[None, :]).astype(np.float32)
    cmfull = np.repeat(cm[:, :, None], 128, axis=2).reshape(128, 1024)
    sm = np.ones((128, 512), np.float32)
    sm[:, ::16] = 0.0
    cf32 = np.concatenate([ident, colmask, Mf, Mb, bd, cmfull, sm], axis=1)
    sel = np.zeros((32, 32, 128), np.float32)
    for e in range(32):
        sel[e, e, :] = 1.0
    sel = sel.reshape(32, 4096)
    t = np.arange(SL)
    row = (t // 64).astype(np.float32)
    colp = (t % 64).astype(np.float32)
    half = 32
    inv = (np.float32(10000.0) ** (-np.arange(0, half, 2, dtype=np.float32) / np.float32(half))).astype(np.float32)
    ang = np.concatenate([row[:, None] * inv, colp[:, None] * inv], axis=-1).astype(np.float32)
    cos = np.cos(ang).astype(np.float32)
    sin = np.sin(ang).astype(np.float32)
    d = p % 64
    cosT = cos[:, d // 2].T
    sgn = np.where(d % 2 == 0, -1.0, 1.0).astype(np.float32)
    sinT = sin[:, d // 2].T * sgn[:, None]
    rope = np.stack([cosT, sinT], axis=1).astype(np.float32)
    return (np.ascontiguousarray(cbf), np.ascontiguousarray(cf32), np.ascontiguousarray(sel), np.ascontiguousarray(rope))


def make_in_maps(x, c, ctx, c_ctx, w_ada, b_ada, norm1_g, w_in, na_q_norm, na_k_norm, na_rpb,
                 gqa_q_norm, gqa_k_norm, hgrn_lb, hgrn_o_norm, w_out, norm2_g, w_route_group,
                 b_route_group, w_route_expert, b_route_expert, w_exp_gate, w_exp_up, w_exp_down):
    f = lambda a: np.ascontiguousarray(np.asarray(a, dtype=np.float32))
    x, c, ctx, c_ctx = f(x), f(c), f(ctx), f(c_ctx)
    cbf, cf32, sel, rope = _consts()
    b_adaT = f(np.asarray(b_ada).reshape(NL, 48, 128).transpose(0, 2, 1))
    rep2 = lambda v: np.concatenate([np.asarray(v), np.asarray(v)], axis=1)
    vecs = np.concatenate([
        np.asarray(norm1_g).reshape(NL, 8, 128).transpose(0, 2, 1),
        np.asarray(norm2_g).reshape(NL, 8, 128).transpose(0, 2, 1),
        rep2(na_q_norm)[:, :, None], rep2(na_k_norm)[:, :, None], rep2(gqa_q_norm)[:, :, None],
        rep2(gqa_k_norm)[:, :, None], rep2(hgrn_o_norm)[:, :, None]], axis=2)
    vecs = f(vecs)
    lbraw = f(np.asarray(hgrn_lb).reshape(NL, 2, 2, 128).transpose(3, 0, 1, 2).reshape(128, 8))
    ck = np.arange(128) % 64
    cq = np.arange(64)
    dc = np.clip(ck[:, None] - cq[None, :] + 15, 0, 30)
    nab = np.asarray(na_rpb)[:, :, :, dc]
    nab = f(nab.transpose(0, 3, 1, 2, 4).reshape(NL, 128, 3840))
    wr = f(np.concatenate([np.asarray(w_route_group), np.asarray(w_route_expert)], axis=2))
    br = np.concatenate([np.asarray(b_route_group), np.asarray(b_route_expert)], axis=1)
    brb = f(np.broadcast_to(br[:, None, :], (NL, 128, 36)))
    shared = dict(cbf=cbf, cf32=cf32, sel=sel, rope=rope, w_ada=f(w_ada), b_adaT=b_adaT, vecs=vecs, lbraw=lbraw,
                  w_in=f(w_in), nab=nab, w_out=f(w_out), wr=wr, brb=brb,
                  w_eg=f(np.asarray(w_exp_gate).reshape(NL, 32, 8, 128, 256).transpose(0, 1, 3, 2, 4).reshape(NL, 32, 128, 2048)),
                  w_eu=f(np.asarray(w_exp_up).reshape(NL, 32, 8, 128, 256).transpose(0, 1, 3, 2, 4).reshape(NL, 32, 128, 2048)),
                  w_ed=f(np.asarray(w_exp_down).reshape(NL, 32, 2, 128, 1024).transpose(0, 1, 3, 2, 4).reshape(NL, 32, 128, 2048)))
    maps = []
    for b in range(x.shape[0]):
        xT = np.ascontiguousarray(np.concatenate([x[b].T, ctx[b].T], axis=1))
        cv = np.stack([c[b].reshape(8, 128).T, c_ctx.reshape(8, 128).T], axis=2).reshape(128, 16)
        m = dict(shared)
        m['xT_in'] = xT
        m['cvec'] = np.ascontiguousarray(cv.astype(np.float32))
        maps.append(m)
    return maps


_NC_CACHE = {}


def kernel(**inputs):
    maps = make_in_maps(**inputs)
    if 'nc' not in _NC_CACHE:
        _NC_CACHE['nc'] = build_nc()[0]
    nc = _NC_CACHE['nc']
    res = run_bass_kernel_spmd(nc, maps, core_ids=list(range(8)))
    out = np.stack([np.ascontiguousarray(r["outT"].T) for r in res.results], axis=0)
    return out.astype(np.float32)
```

```python
import contextlib
import numpy as np
import concourse.bass as bass
import concourse.mybir as mybir
from concourse.bass_utils import run_bass_kernel_spmd

F32 = mybir.dt.float32
BF16 = mybir.dt.bfloat16
AF = mybir.ActivationFunctionType
ALU = mybir.AluOpType
AX = mybir.AxisListType

NL = 2
D = 1024
SL = 2048
LC = 256
T = SL + LC
EPS = 1e-6
BLK = [(0, 512), (512, 1024), (1024, 1536), (1536, 2048), (2048, 2304)]
ENGS = ('pe', 'act', 'dve', 'pool', 'sp')


class Sched:
    def __init__(self, nc):
        self.nc = nc
        self.ops = []
        self.last_w = {}
        self.readers = {}
        self.dma_streams = []

    def op(self, eng, fn, r=(), w=(), dma=None):
        deps = set()
        for k in r:
            lw = self.last_w.get(k)
            if lw is not None:
                deps.add(lw)
        for k in w:
            lw = self.last_w.get(k)
            if lw is not None:
                deps.add(lw)
            deps.update(self.readers.get(k, ()))
        i = len(self.ops)
        if dma is not None:
            stream = ('d', dma)
            if stream not in self.dma_streams:
                self.dma_streams.append(stream)
        else:
            stream = ('e', eng)
        self.ops.append(dict(eng=eng, fn=fn, deps=deps, stream=stream))
        for k in r:
            self.readers.setdefault(k, []).append(i)
        for k in w:
            self.last_w[k] = i
            self.readers[k] = []
        return i

    def barrier(self):
        last = {}
        for i, o in enumerate(self.ops):
            last[o['stream']] = i
        ids = set(last.values())
        for e in ENGS:
            i = self.op(e, None)
            self.ops[i]['deps'] = set(ids)
        self.last_w = {}
        self.readers = {}

    def emit(self):
        nc = self.nc
        ops = self.ops
        spos = {}
        for o in ops:
            s = o['stream']
            o['pos'] = spos.get(s, 0)
            spos[s] = o['pos'] + 1
        known = {e: {} for e in ENGS}
        last_in_stream = {}
        for i, o in enumerate(ops):
            E = o['eng']
            kn = known[E]
            waits = []
            for d in sorted(o['deps']):
                Dd = ops[d]
                sD = Dd['stream']
                if sD == ('e', 'pe') and E == 'pe' and o['stream'][0] == 'e':
                    continue
                if kn.get(sD, -1) >= Dd['pos']:
                    continue
                if sD[0] == 'd':
                    Dd = ops[last_in_stream[sD]]
                Dd['sig'] = True
                waits.append(Dd)
                for s, p in Dd['clock'].items():
                    if kn.get(s, -1) < p:
                        kn[s] = p
            o['waits'] = waits
            ck = dict(kn)
            ck[o['stream']] = o['pos']
            o['clock'] = ck
            last_in_stream[o['stream']] = i
            if o['stream'][0] == 'd':
                o['sig'] = True
        cnt = {}
        for o in ops:
            s = o['stream']
            if o.get('sig'):
                inc = 16 if s[0] == 'd' else 1
                cnt[s] = cnt.get(s, 0) + inc
                o['sigval'] = cnt[s]
                o['inc'] = inc
        self.stats = dict(n_ops=len(ops), sig=dict(cnt))
        streams = [('e', e) for e in ENGS] + self.dma_streams
        with contextlib.ExitStack() as st:
            sems = {}
            for s in streams:
                sems[s] = st.enter_context(nc.semaphore("s_%s_%s" % (s[0], str(s[1]))))
            block = st.enter_context(nc.Block())

            def run(E):
                def body(eobj):
                    for o in ops:
                        if o['eng'] != E:
                            continue
                        best = {}
                        for Dd in o['waits']:
                            s = Dd['stream']
                            if best.get(s, 0) < Dd['sigval']:
                                best[s] = Dd['sigval']
                        for s, v in best.items():
                            eobj.wait_ge(sems[s], v)
                        if o['fn'] is None:
                            if o.get('sig'):
                                eobj.nop().then_inc(sems[o['stream']], o['inc'])
                            continue
                        ins = o['fn'](eobj)
                        if o.get('sig'):
                            ins.then_inc(sems[o['stream']], o['inc'])
                return body

            block.tensor(run('pe'))
            block.scalar(run('act'))
            block.vector(run('dve'))
            block.gpsimd(run('pool'))
            block.sync(run('sp'))


class _Stop(Exception):
    pass


def build_nc(n_layers=NL, dbg=(), stop=None):
    nc = bass.Bass("TRN2", target_bir_lowering=False)
    S = Sched(nc)

    def din(name, shape, dt=F32):
        return nc.dram_tensor(name, list(shape), dt, kind="ExternalInput").ap()

    xT_in = din("xT_in", [D, T])
    cvec_d = din("cvec", [128, 16])
    cbf_d = din("cbf", [128, 768])
    cf32_d = din("cf32", [128, 2112])
    sel_d = din("sel", [32, 4096])
    rope_d = din("rope", [128, 2, SL])
    w_ada_d = din("w_ada", [NL, D, 6 * D])
    b_ada_d = din("b_adaT", [NL, 128, 48])
    vecs_d = din("vecs", [NL, 128, 21])
    lbraw_d = din("lbraw", [128, 8])
    w_in_d = din("w_in", [NL, D, 2816])
    nab_d = din("nab", [NL, 128, 3840])
    w_out_d = din("w_out", [NL, D, D])
    wr_d = din("wr", [NL, D, 36])
    brb_d = din("brb", [NL, 128, 36])
    weg_d = din("w_eg", [NL, 32, 128, 2048])
    weu_d = din("w_eu", [NL, 32, 128, 2048])
    wed_d = din("w_ed", [NL, 32, 128, 2048])
    outT = nc.dram_tensor("outT", [D, SL], F32, kind="ExternalOutput").ap()
    hT_d = nc.dram_tensor("hT_scr", [5, 128, 4096], BF16, kind="Internal").ap()
    comb_d = nc.dram_tensor("comb_scr", [32, T], F32, kind="Internal").ap()
    dbg_out = {}

    st = contextlib.ExitStack()

    def sb(name, shape, dt=F32):
        return st.enter_context(nc.sbuf_tensor("s_" + name, list(shape), dt))

    psf = [st.enter_context(nc.psum_tensor("psf%d" % i, [128, 512], F32)) for i in range(8)]
    psb = [psf[6][:].bitcast(BF16), psf[7][:].bitcast(BF16)]
    ring_state = {'i': 0}

    def nextps():
        i = ring_state['i']
        ring_state['i'] = (i + 1) % 4
        return psf[i], 'psf%d' % i

    rings = {}

    def ring(name, n):
        i = rings.get(name, 0)
        rings[name] = (i + 1) % n
        return i

    XT = sb("XT", [128, 8, T])
    cbf = sb("cbf", [128, 768], BF16)
    cf32 = sb("cf32", [128, 2112])
    cvec = sb("cvec", [128, 8, 2])
    scv = sb("scv", [128, 8, 2])
    modT = sb("modT", [128, 48, 2])
    vecs = sb("vecs", [128, 21])
    A1 = sb("A1", [128, 8, 2])
    A2 = sb("A2", [128, 8, 2])
    lbs = sb("lbs", [128, 8])
    lbv = sb("lbv", [128, 8])
    onesf = sb("onesf", [128, 64])

    ident_bf = cbf[:, 0:128]
    swap_bf = cbf[:, 128:256]
    onesbd_bf = cbf[:, 256:384]
    ones_bf = cbf[:, 384:512]
    oneshalf = [cbf[:, 512:640], cbf[:, 640:768]]
    ident_f = cf32[:, 0:128]
    colmask = cf32[:, 128:192]
    Mdir = [cf32[:, 192:320], cf32[:, 320:448]]
    bdm = cf32[:, 448:576]
    cmfull = cf32[:, 576:1600]
    scanmask = cf32[:, 1600:2112]

    def mm(out, lhsT, rhs, start, stop, r, w):
        S.op('pe', lambda e: e.matmul(out, lhsT, rhs, start=start, stop=stop), r=r, w=w)

    def act(out, in_, func, r, w, scale=1.0, bias=0.0):
        S.op('act', lambda e: e.activation(out=out, in_=in_, func=func, bias=bias, scale=scale), r=r, w=w)

    def tt(eng, out, in0, in1, op, r, w):
        S.op(eng, lambda e: e.tensor_tensor(out=out, in0=in0, in1=in1, op=op), r=r, w=w)

    def ts(eng, out, in0, s1, s2, op0, op1, r, w):
        if s2 is None:
            S.op(eng, lambda e: e.tensor_scalar(out=out, in0=in0, scalar1=s1, scalar2=None, op0=op0), r=r, w=w)
        else:
            S.op(eng, lambda e: e.tensor_scalar(out=out, in0=in0, scalar1=s1, scalar2=s2, op0=op0, op1=op1), r=r, w=w)

    def stt(eng, out, in0, scalar, in1, op0, op1, r, w):
        S.op(eng, lambda e: e.scalar_tensor_tensor(out=out, in0=in0, scalar=scalar, in1=in1, op0=op0, op1=op1), r=r, w=w)

    def cp(eng, out, in_, r, w):
        if eng == 'act':
            act(out, in_, AF.Copy, r, w)
        else:
            S.op(eng, lambda e: e.tensor_copy(out=out, in_=in_), r=r, w=w)

    def recip(out, in_, r, w):
        S.op('dve', lambda e: e.reciprocal(out=out, in_=in_), r=r, w=w)

    def memset(eng, ap, val, w):
        S.op(eng, lambda e: e.memset(ap, val), w=w)

    def dma(eng, out, in_, r, w, stream):
        S.op(eng, lambda e: e.dma_start(out=out, in_=in_), r=r, w=w, dma=stream)

    def dump(name, ap, keys, shape, dt=F32):
        if name not in dbg:
            return
        d = nc.dram_tensor("dbg_" + name, list(shape), dt, kind="ExternalOutput").ap()
        dbg_out[name] = d
        dma('sp', d, ap, keys, ['dbg_' + name], 'dbg')

    dma('pool', cbf[:], cbf_d, [], ['cbf'], 'c0')
    dma('sp', cf32[:], cf32_d, [], ['cf32'], 'c1')
    dma('sp', cvec[:], cvec_d.rearrange("p (k j) -> p k j", j=2), [], ['cvec'], 'c2')
    dma('sp', lbs[:], lbraw_d, [], ['lbs'], 'c3')
    memset('pool', onesf[:], 1.0, ['onesf'])
    xv = xT_in.rearrange("(c p) t -> p c t", p=128)
    for c in range(8):
        dma('sp', XT[:, c, :], xv[:, c, :], [], [('XT', b) for b in range(5)], 'x%d' % (c % 4))
    act(scv[:], cvec[:], AF.Exp, ['cvec'], ['scv'], scale=-1.0)
    ts('dve', scv[:], scv[:], 1.0, None, ALU.add, None, ['scv'], ['scv'])
    recip(scv[:], scv[:], ['scv'], ['scv'])
    tt('dve', scv[:], scv[:], cvec[:], ALU.mult, ['scv', 'cvec'], ['scv'])
    lb1 = sb("lb1", [128, 4])
    tt('dve', lb1[:], lbs[:, 0:4], lbs[:, 4:8], ALU.subtract, ['lbs'], ['lb1'])
    act(lb1[:], lb1[:], AF.Exp, ['lb1'], ['lb1'])
    ts('dve', lb1[:], lb1[:], 1.0, None, ALU.add, None, ['lb1'], ['lb1'])
    recip(lb1[:], lb1[:], ['lb1'], ['lb1'])

    XTk = lambda b: [('XT', b)]

    def stage(k):
        if stop == k:
            raise _Stop()

    def layer(l):
        last = (l == NL - 1)
        nblk = 4 if last else 5
        S.barrier()
        dma('sp', vecs[:], vecs_d[l], [], ['vecs'], 'c2')
        if l == 0:
            memset('pool', lbv[:, 0:4], 1.0, ['lbv'])
            memset('pool', lbv[:, 4:8], 1e-20, ['lbv'])
        else:
            ts('dve', lbv[:, 0:4], lb1[:], -1.0, 1.0, ALU.mult, ALU.add, ['lb1'], ['lbv'])
            ts('dve', lbv[:, 4:8], lb1[:], 1e-20, None, ALU.max, None, ['lb1'], ['lbv'])
        with contextlib.ExitStack() as st2:
            wa = [st2.enter_context(nc.sbuf_tensor("wa%d_%d" % (i, l), [128, 8, 1024], F32)) for i in range(2)]
            badT = st2.enter_context(nc.sbuf_tensor("badT_%d" % l, [128, 48], F32))
            dma('sp', badT[:], b_ada_d[l], [], ['badT'], 'c3')
            wav = w_ada_d[l].rearrange("(kc p) n -> p kc n", p=128)
            psm = psf[4]
            for m in range(6):
                wt = wa[m % 2]
                dma('sp', wt[:], wav[:, :, m * 1024:(m + 1) * 1024], [], ['wa%d' % (m % 2)], 'wa%d' % (m % 2))
                for j in range(8):
                    for kc in range(8):
                        mm(psm[:, (m * 8 + j) * 2:(m * 8 + j) * 2 + 2], wt[:, kc, j * 128:(j + 1) * 128], scv[:, kc, :],
                           kc == 0, kc == 7, ['wa%d' % (m % 2), 'scv'], ['psm'])
            tt('dve', modT[:], psm[:, 0:96].rearrange("p (a b) -> p a b", b=2),
               badT[:].unsqueeze(2).to_broadcast([128, 48, 2]), ALU.add, ['psm', 'badT'], ['modT'])
        S.barrier()
        stage(0)
        stt('dve', A1[:], modT[:, 8:16, :], 1.0, vecs[:, 0:8].unsqueeze(2).to_broadcast([128, 8, 2]),
            ALU.add, ALU.mult, ['modT', 'vecs'], ['A1'])
        stt('dve', A2[:], modT[:, 32:40, :], 1.0, vecs[:, 8:16].unsqueeze(2).to_broadcast([128, 8, 2]),
            ALU.add, ALU.mult, ['modT', 'vecs'], ['A2'])

        def modv(m, c, col):
            return modT[:, m * 8 + c, col:col + 1]

        stm = contextlib.ExitStack()
        U = {}
        wph = stm.enter_context(nc.sbuf_tensor("wph_%d" % l, [128, 8, 640], BF16))

        def unit_bufs(st_, tag, nhb, nch):
            U['hb'] = [st_.enter_context(nc.sbuf_tensor("hb%d_%s" % (i, tag), [128, 8, 512], BF16)) for i in range(nhb)]
            U['woutu'] = st_.enter_context(nc.sbuf_tensor("woutu_%s" % tag, [128, nch, D], BF16))
            U['mixu'] = st_.enter_context(nc.sbuf_tensor("mixu_%s" % tag, [128, nch, T], BF16))
            rings['hb'] = 0
            pend_h.clear()

        def norm_block(b, Asc, mshift, tmp, sqb, rstd, sfx='', sfx_sq=None):
            t0, t1 = BLK[b]
            n = t1 - t0
            col = 0 if b < 4 else 1
            ksq = 'sqb' + (sfx if sfx_sq is None else sfx_sq)
            krs, ktm = 'rstd' + sfx, 'ntmp' + sfx
            act(sqb[:, :, :n], XT[:, :, t0:t1], AF.Square, XTk(b), [ksq])
            ps, pk = nextps()
            for c in range(8):
                mm(ps[:, :n], ones_bf, sqb[:, c, :n], c == 0, c == 7, [ksq, 'cbf'], [pk])
            act(rstd[:, :n], ps[:, :n], AF.Ln, [pk], [krs], scale=1.0 / D, bias=EPS)
            act(rstd[:, :n], rstd[:, :n], AF.Exp, [krs], [krs], scale=-0.5)
            tt('dve', tmp[:, :, :n], XT[:, :, t0:t1], rstd[:, :n].unsqueeze(1).to_broadcast([128, 8, n]), ALU.mult,
               XTk(b) + [krs], [ktm])
            for c in range(8):
                act(tmp[:, c, :n], tmp[:, c, :n], AF.Identity, [ktm, 'modT', Asc[1]], [ktm],
                    scale=Asc[0][:, c, col:col + 1], bias=modv(mshift, c, col))

        with contextlib.ExitStack() as st2:
            tmp2 = [st2.enter_context(nc.sbuf_tensor("ntmp%d_%d" % (i, l), [128, 8, 512], F32)) for i in range(2)]
            sqb2 = [st2.enter_context(nc.sbuf_tensor("sqb%d_%d" % (i, l), [128, 8, 512], BF16)) for i in range(2)]
            rstd2 = [st2.enter_context(nc.sbuf_tensor("rstd%d_%d" % (i, l), [128, 512], F32)) for i in range(2)]
            hb_t = [st2.enter_context(nc.sbuf_tensor("hbn%d_%d" % (i, l), [128, 8, 512], BF16)) for i in range(2)]
            for b in range(5):
                t0, t1 = BLK[b]
                n = t1 - t0
                x_ = b % 2
                norm_block(b, (A1, 'A1'), 0, tmp2[x_], sqb2[x_], rstd2[x_], str(x_))
                hb = hb_t[x_]
                cp('pool', hb[:, :, :n], tmp2[x_][:, :, :n], ['ntmp%d' % x_], ['hb%d' % x_])
                dma('sp', hT_d[b].rearrange("p (c t) -> p c t", t=512)[:, :, :n], hb[:, :, :n], ['hb%d' % x_], [('hT', b)], 'hs')
        S.barrier()

        stage(1)

        def load_h(b):
            t0, t1 = BLK[b]
            i = ring('hb', len(U['hb']))
            dma('sp', U['hb'][i][:, :, :t1 - t0], hT_d[b].rearrange("p (c t) -> p c t", t=512)[:, :, :t1 - t0], [('hT', b)], ['hb%d' % i], 'hl%d' % i)
            return U['hb'][i], 'hb%d' % i

        pend_h = {}

        def get_h(b):
            if b in pend_h:
                return pend_h.pop(b)
            return load_h(b)

        def prefetch_h(b):
            if b not in pend_h:
                pend_h[b] = load_h(b)

        win = w_in_d[l].rearrange("(kc p) n -> p kc n", p=128)

        def load_w(dst0, src0, ncol, first):
            dma('pool', wph[:, :, dst0:dst0 + ncol], win[:, :, src0:src0 + ncol], [], ['wph'], 'wph')

        def proj_fm(wcol, b, hb, hk):
            t0, t1 = BLK[b]
            n = t1 - t0
            ps, pk = nextps()
            for kc in range(8):
                mm(ps[:, :n], wph[:, kc, wcol:wcol + 128], hb[:, kc, :n], kc == 0, kc == 7, ['wph', hk], [pk])
            return ps, pk

        def proj_tm(wcol, ncol, hb, hk, tl):
            ps, pk = nextps()
            for kc in range(8):
                mm(ps[:, :ncol], hb[:, kc, tl * 128:(tl + 1) * 128], wph[:, kc, wcol:wcol + ncol], kc == 0, kc == 7, ['wph', hk], [pk])
            return ps, pk

        def head_norm(ps, pk, n, gcol, sqh, rs, qn):
            act(sqh[:, :n], ps[:, :n], AF.Square, [pk], ['sqh'])
            p2, p2k = nextps()
            mm(p2[:, :n], onesbd_bf, sqh[:, :n], True, True, ['sqh', 'cbf'], [p2k])
            act(rs[:, :n], p2[:, :n], AF.Ln, [p2k], ['rs'], scale=1.0 / 64, bias=EPS)
            act(rs[:, :n], rs[:, :n], AF.Exp, ['rs'], ['rs'], scale=-0.5)
            stt('dve', qn[:, :n], ps[:, :n], vecs[:, gcol:gcol + 1], rs[:, :n], ALU.mult, ALU.mult, [pk, 'rs', 'vecs'], ['qn'])

        def wout_load(nch, row0):
            wov_ = w_out_d[l].rearrange("(kc p) n -> p kc n", p=128)
            dma('pool', U['woutu'][:, 0:nch, :], wov_[:, row0:row0 + nch, :], [], ['woutu'], 'wo')

        def wout_partial(nch, row0, blocks):
            wov = w_out_d[l].rearrange("(kc p) n -> p kc n", p=128)
            woutu, mixu = U['woutu'], U['mixu']
            for b in blocks:
                t0, t1 = BLK[b]
                n = t1 - t0
                col = 0 if b < 4 else 1
                for dc in range(8):
                    ps, pk = nextps()
                    for kc in range(nch):
                        mm(ps[:, :n], woutu[:, kc, dc * 128:(dc + 1) * 128], mixu[:, kc, t0:t1], kc == 0, kc == nch - 1,
                           ['woutu', ('mixu', b)], [pk])
                    stt('dve', XT[:, dc, t0:t1], ps[:, :n], modv(2, dc, col), XT[:, dc, t0:t1], ALU.mult, ALU.add,
                        [pk, 'modT'] + XTk(b), XTk(b))

        for pr in range(2):
            with contextlib.ExitStack() as st2:
                def sb2(name, shape, dt=F32):
                    return st2.enter_context(nc.sbuf_tensor(name + "_a%d%d" % (l, pr), list(shape), dt))
                unit_bufs(st2, "a%d%d" % (l, pr), 2, 1)
                mixu = U['mixu']
                aq = sb2("aq", [128, T], BF16)
                akz = [sb2("akz%d" % i, [128, T], BF16) for i in range(2)]
                avp = sb2("avp", [128, 18, 2, 128], BF16)
                nabm = sb2("nabm", [128, 2, 15, 64])
                PTa = [sb2("PTa%d" % i, [128, 7, 256], BF16) for i in range(2)]
                sqh = sb2("sqh", [128, 512], BF16)
                rs = sb2("rs", [128, 512])
                qn = sb2("qn", [128, 512])
                rden = [sb2("rden%d" % i, [128, 128]) for i in range(2)]
                load_w(0, pr * 128, 128, True)
                load_w(128, 256 + pr * 128, 128, False)
                load_w(256, 512 + pr * 128, 128, False)
                wout_load(1, pr)
                dma('sp', nabm[:].rearrange("p a b c -> p (a b c)"), nab_d[l][:, pr * 1920:(pr + 1) * 1920], [], ['nabm'], 'c3')
                tt('dve', nabm[:].rearrange("p a b c -> p (a b) c"), nabm[:].rearrange("p a b c -> p (a b) c"),
                   colmask.unsqueeze(1).to_broadcast([128, 30, 64]), ALU.add, ['nabm', 'cf32'], ['nabm'])
                memset('pool', avp[:], 0.0, ['avp'])
                for i in range(2):
                    memset('pool', akz[i][:], 0.0, [('ak', b) for b in range(5)])
                for b in range(5):
                    t0, t1 = BLK[b]
                    n = t1 - t0
                    hb, hk = get_h(b)
                    if b + 1 < 5:
                        prefetch_h(b + 1)
                    if not (b == 4 and last):
                        ps, pk = proj_fm(0, b, hb, hk)
                        head_norm(ps, pk, n, 16, sqh, rs, qn)
                        cp('act', aq[:, t0:t1], qn[:, :n], ['qn'], [('aq', b)])
                    ps, pk = proj_fm(128, b, hb, hk)
                    head_norm(ps, pk, n, 17, sqh, rs, qn)
                    cp('act', akz[0][0:64, t0:t1], qn[0:64, :n], ['qn'], [('ak', b)])
                    cp('act', akz[1][64:128, t0:t1], qn[64:128, :n], ['qn'], [('ak', b)])
                    for tl in range(n // 128):
                        tg = t0 // 128 + tl
                        ps, pk = proj_tm(256, 128, hb, hk, tl)
                        cp('act', avp[:, tg, 0, 0:64], ps[:, 0:64], [pk], ['avp'])
                        cp('dve', avp[:, tg, 1, 64:128], ps[:, 64:128], [pk], ['avp'])
                stage(20)
                qtiles = list(range(16)) + ([] if last else [16, 17])
                na_kts = {}
                cnts = {'s': 0, 'b': 0}

                def na_subs(qt, kt):
                    subs = []
                    for kh in range(2):
                        for qh in range(2):
                            kr = 2 * kt + kh
                            qr = 2 * qt + qh
                            rs0 = min(max(qr - 4, 0), 24)
                            if rs0 <= kr < rs0 + 8:
                                subs.append((kh, qh, kr - qr + 7))
                    return tuple(subs)

                classes = {}
                for qt_ in range(16):
                    for kt_ in range(16):
                        sb_ = na_subs(qt_, kt_)
                        if sb_ and sb_ not in classes:
                            classes[sb_] = len(classes)
                ncls = len(classes)
                EN = sb2("EN", [128, 2, 15, 64], BF16)
                EB = sb2("EB", [128, ncls, 256], BF16)
                PTr = [sb2("PTr%d" % i, [128, 256], BF16) for i in range(3)]
                act(EN[:].rearrange("p a b c -> p (a b c)"), nabm[:].rearrange("p a b c -> p (a b c)"), AF.Exp, ['nabm'], ['EN'])
                memset('pool', EB[:], 0.0, ['EB'])
                for sb_, c_ in classes.items():
                    for (kh, qh, dri) in sb_:
                        o_ap = EB[kh * 64:(kh + 1) * 64, c_, :].rearrange("p (h q) -> p h q", h=2)[:, :, qh * 64:(qh + 1) * 64]
                        cp('dve', o_ap, EN[kh * 64:(kh + 1) * 64, :, dri, :], ['EN'], ['EB'])

                def na_A(qi):
                    qt = qtiles[qi]
                    qb = qt // 4 if qt < 16 else 4
                    kts = []
                    if qt < 16:
                        for kt in range(16):
                            subs = na_subs(qt, kt)
                            if subs:
                                kts.append((kt, subs))
                    kts += [(16, None), (17, None)]
                    assert len(kts) <= 7
                    na_kts[qi] = kts
                    pt = PTa[qi % 2]
                    for j, (kt, subs) in enumerate(kts):
                        kb = kt // 4 if kt < 16 else 4
                        bi = cnts['s'] % 4
                        cnts['s'] += 1
                        ps, pk = psf[bi], 'psf%d' % bi
                        for hh in range(2):
                            mm(ps[:, hh * 128:(hh + 1) * 128], akz[hh][:, kt * 128:(kt + 1) * 128],
                               aq[:, qt * 128:(qt + 1) * 128], True, True, [('ak', kb), ('aq', qb)], [pk])
                        if subs is None:
                            act(pt[:, j, :], ps[:, 0:256], AF.Exp, [pk], [('PTa', qi % 2, j)], scale=0.125)
                        else:
                            si = cnts['b'] % 3
                            cnts['b'] += 1
                            act(PTr[si][:], ps[:, 0:256], AF.Exp, [pk], ['PTr%d' % si], scale=0.125)
                            tt('dve', pt[:, j, :], PTr[si][:], EB[:, classes[subs], :], ALU.mult,
                               ['PTr%d' % si, 'EB'], [('PTa', qi % 2, j)])

                def na_B(qi):
                    qt = qtiles[qi]
                    qb = qt // 4 if qt < 16 else 4
                    kts = na_kts[qi]
                    pt = PTa[qi % 2]
                    nk = len(kts)
                    bo = 4 + 2 * (qi % 2)
                    pO, pD = psf[bo], psf[bo + 1]
                    kO, kD = 'psf%d' % bo, 'psf%d' % (bo + 1)
                    cnt = 0
                    for hh in range(2):
                        for j, (kt, subs) in enumerate(kts):
                            mm(pO[:, 0:128], avp[:, kt, hh, :], pt[:, j, hh * 128:(hh + 1) * 128], cnt == 0, cnt == 2 * nk - 1,
                               ['avp', ('PTa', qi % 2, j)], [kO])
                            cnt += 1
                    cnt = 0
                    for hh in range(2):
                        for j, (kt, subs) in enumerate(kts):
                            mm(pD[:, 0:128], oneshalf[hh], pt[:, j, hh * 128:(hh + 1) * 128], cnt == 0, cnt == 2 * nk - 1,
                               ['cbf', ('PTa', qi % 2, j)], [kD])
                            cnt += 1
                    ri = qi % 2
                    recip(rden[ri][:], pD[:, 0:128], [kD], ['rden%d' % ri])
                    tt('dve', mixu[:, 0, qt * 128:(qt + 1) * 128], pO[:, 0:128], rden[ri][:], ALU.mult, [kO, 'rden%d' % ri], [('mixu', qb)])

                nq = len(qtiles)
                for t_ in range(nq + 1):
                    if t_ < nq:
                        na_A(t_)
                    if t_ >= 1:
                        na_B(t_ - 1)
                if 'mixA%d' % pr in dbg and l == 0:
                    dump('mixA%d' % pr, mixu[:, 0, :], [('mixu', b) for b in range(5)], [128, T], BF16)
                wout_partial(1, pr, range(nblk))
            S.barrier()

        stage(2)
        for g in range(2):
            with contextlib.ExitStack() as st2:
                def sb2(name, shape, dt=F32):
                    return st2.enter_context(nc.sbuf_tensor(name + "_b%d%d" % (l, g), list(shape), dt))
                unit_bufs(st2, "b%d%d" % (l, g), 2, 2)
                mixu = U['mixu']
                bq = sb2("bq", [128, 2, T], BF16)
                kkz = [sb2("kkz%d" % i, [128, T], BF16) for i in range(2)]
                vpd = sb2("vpd", [128, 18, 2, 128], BF16)
                ropet = sb2("ropet", [128, 2, SL])
                PT = [sb2("PT%d" % i, [128, 512], BF16) for i in range(4)]
                sqh = sb2("sqh", [128, 512], BF16)
                rs = sb2("rs", [128, 512])
                qn = sb2("qn", [128, 512])
                qnb = sb2("qnb", [128, 512], BF16)
                r1 = sb2("r1", [128, 512])
                r2 = sb2("r2", [128, 512])
                rden = [sb2("rden%d" % i, [128, 512]) for i in range(2)]
                dma('sp', ropet[:], rope_d, [], ['ropet'], 'c3')
                load_w(0, 768 + g * 256, 256, True)
                load_w(256, 1280 + g * 64, 64, False)
                load_w(320, 1280 + g * 64, 64, False)
                load_w(384, 1408 + g * 64, 64, False)
                wout_load(2, 2 + 2 * g)
                memset('pool', vpd[:], 0.0, ['vpd'])
                for i in range(2):
                    memset('pool', kkz[i][:], 0.0, [('kk', b) for b in range(5)])
                for b in range(5):
                    t0, t1 = BLK[b]
                    n = t1 - t0
                    hb, hk = get_h(b)
                    if b + 1 < 5:
                        prefetch_h(b + 1)
                    for ch in range(3):
                        if ch < 2 and b == 4 and last:
                            continue
                        ps, pk = proj_fm(ch * 128, b, hb, hk)
                        head_norm(ps, pk, n, 18 if ch < 2 else 19, sqh, rs, qn)
                        dk = ('bq', ch, b) if ch < 2 else ('kk', b)
                        if ch < 2:
                            dsts = [(bq[:, ch, t0:t1], slice(0, 128))]
                        else:
                            dsts = [(kkz[0][0:64, t0:t1], slice(0, 64)), (kkz[1][64:128, t0:t1], slice(64, 128))]
                        if b == 4:
                            for dst, sl in dsts:
                                cp('act', dst, qn[sl, :n], ['qn'], [dk])
                        else:
                            cp('act', qnb[:, :n], qn[:, :n], ['qn'], ['qnb'])
                            p2, p2k = nextps()
                            mm(p2[:, :n], swap_bf, qnb[:, :n], True, True, ['qnb', 'cbf'], [p2k])
                            tt('pool', r1[:, :n], qn[:, :n], ropet[:, 0, t0:t1], ALU.mult, ['qn', 'ropet'], ['r1'])
                            tt('dve', r2[:, :n], p2[:, :n], ropet[:, 1, t0:t1], ALU.mult, [p2k, 'ropet'], ['r2'])
                            for dst, sl in dsts:
                                tt('dve', dst, r1[sl, :n], r2[sl, :n], ALU.add, ['r1', 'r2'], [dk])
                    for tl in range(n // 128):
                        tg = t0 // 128 + tl
                        ps, pk = proj_tm(384, 64, hb, hk, tl)
                        cp('act', vpd[:, tg, 0, 0:64], ps[:, 0:64], [pk], ['vpd'])
                        cp('dve', vpd[:, tg, 1, 64:128], ps[:, 0:64], [pk], ['vpd'])
                items = []
                gi = 0
                for qb in range(nblk):
                    kts = list(range(18)) if qb < 4 else [16, 17]
                    for ch in range(2):
                        lst = [(hh, kt) for hh in range(2) for kt in kts]
                        for j, (hh, kt) in enumerate(lst):
                            items.append((gi, qb, ch, hh, kt, j == 0, j == len(lst) - 1))
                        gi += 1

                def gq_A(i):
                    gi, qb, ch, hh, kt, first, lastk = items[i]
                    t0, t1 = BLK[qb]
                    n = t1 - t0
                    kb = kt // 4 if kt < 16 else 4
                    bi = i % 4
                    ps, pk = psf[bi], 'psf%d' % bi
                    mm(ps[:, :n], kkz[hh][:, kt * 128:(kt + 1) * 128], bq[:, ch, t0:t1],
                       True, True, [('kk', kb), ('bq', ch, qb)], [pk])
                    act(PT[bi][:, :n], ps[:, :n], AF.Exp, [pk], ['PT%d' % bi], scale=0.125)

                def gq_B(i):
                    gi, qb, ch, hh, kt, first, lastk = items[i]
                    t0, t1 = BLK[qb]
                    n = t1 - t0
                    bi = i % 4
                    bo = 4 + 2 * (gi % 2)
                    pO, pD = psf[bo], psf[bo + 1]
                    kO, kD = 'psf%d' % bo, 'psf%d' % (bo + 1)
                    mm(pO[:, :n], vpd[:, kt, hh, :], PT[bi][:, :n], first, lastk, ['vpd', 'PT%d' % bi], [kO])
                    mm(pD[:, :n], oneshalf[hh], PT[bi][:, :n], first, lastk, ['cbf', 'PT%d' % bi], [kD])
                    if lastk:
                        ri = gi % 2
                        recip(rden[ri][:, :n], pD[:, :n], [kD], ['rden%d' % ri])
                        tt('dve', mixu[:, ch, t0:t1], pO[:, :n], rden[ri][:, :n], ALU.mult, [kO, 'rden%d' % ri], [('mixu', qb)])

                ni = len(items)
                for t_ in range(ni + 2):
                    if t_ < ni:
                        gq_A(t_)
                    if t_ >= 2:
                        gq_B(t_ - 2)
                if 'mixB%d' % g in dbg and l == 0:
                    dump('mixB%d' % g, mixu[:, :, :], [('mixu', b) for b in range(5)], [128, 2, T], BF16)
                wout_partial(2, 2 + 2 * g, range(nblk))
            S.barrier()

        stage(3)
        for pr in range(2):
            with contextlib.ExitStack() as st2:
                def sb2(name, shape, dt=F32):
                    return st2.enter_context(nc.sbuf_tensor(name + "_c%d%d" % (l, pr), list(shape), dt))
                qs = sb2("qs", [128, T], BF16)
                unit_bufs(st2, "c%d%d" % (l, pr), 1, 1)
                mixu = U['mixu']
                vpc = sb2("vpc", [128, 18, 2, 128], BF16)
                sgate = sb2("sgate", [128, T], BF16)
                osum = sb2("osum", [128, T])
                t1b = sb2("t1b", [128, 512])
                t2b = sb2("t2b", [128, 512])
                t3b = sb2("t3b", [128, 512])
                t4b = sb2("t4b", [128, 512])
                t5b = sb2("t5b", [128, 512])
                t6b = sb2("t6b", [128, 512])
                t7b = sb2("t7b", [128, 512])
                ablk = [sb2("ablk%d" % i, [128, 32]) for i in range(2)]
                qh = [sb2("qh%d" % i, [128, 512], BF16) for i in range(2)]
                kcbz = [[sb2("kcbz%d%d" % (i, j), [128, 512], BF16) for j in range(2)] for i in range(2)]
                kbb = [sb2("kbb%d" % i, [128, 512], BF16) for i in range(2)]
                kbT = [[sb2("kbT%d%d" % (i, j), [128, 128], BF16) for j in range(2)] for i in range(2)]
                vblk = [[sb2("vblk%d%d" % (i, j), [128, 8, 128], BF16) for j in range(2)] for i in range(2)]
                U9 = [sb2("U9%d" % i, [128, 128, 9]) for i in range(3)]
                A9 = [sb2("A9%d" % i, [128, 128, 9], BF16) for i in range(2)]
                S9 = [sb2("S9%d" % i, [128, 128, 9], BF16) for i in range(2)]
                PA = [[sb2("PA%d%d" % (i, j), [128, 128], BF16) for j in range(2)] for i in range(3)]
                oi = [sb2("oi%d" % i, [128, 128]) for i in range(2)]
                sqh = qh[0]
                rs = t1b
                load_w(0, 1536 + pr * 128, 128, True)
                load_w(128, 2048 + pr * 128, 128, False)
                load_w(256, 2304 + pr * 128, 128, False)
                load_w(384, 2560 + pr * 128, 128, False)
                load_w(512, 1792 + pr * 128, 128, False)
                wout_load(1, 6 + pr)
                memset('pool', vpc[:], 0.0, ['vpc'])
                for i in range(2):
                    for j in range(2):
                        memset('pool', kcbz[i][j][:], 0.0, ['kcb%d' % i])
                for i in range(2):
                    memset('pool', A9[i][:], 0.0, ['A9%d' % i])
                for i in range(2):
                    for j in range(2):
                        memset('pool', kbT[i][j][:], 0.0, ['kbT%d' % i])
                        memset('pool', vblk[i][j][:], 0.0, ['vblk%d' % i])
                for b in range(5):
                    t0, t1 = BLK[b]
                    n = t1 - t0
                    hb, hk = get_h(b)
                    ps, pk = proj_fm(0, b, hb, hk)
                    act(qs[:, t0:t1], ps[:, :n], AF.Copy, [pk], [('qs', b)], scale=0.125)
                    if not (b == 4 and last):
                        ps, pk = proj_fm(384, b, hb, hk)
                        act(t1b[:, :n], ps[:, :n], AF.Exp, [pk], ['t1b'], scale=-1.0)
                        act(t1b[:, :n], t1b[:, :n], AF.Ln, ['t1b'], ['t1b'], bias=1.0)
                        act(t1b[:, :n], t1b[:, :n], AF.Exp, ['t1b'], ['t1b'], scale=-1.0)
                        tt('dve', sgate[:, t0:t1], t1b[:, :n], ps[:, :n], ALU.mult, ['t1b', pk], [('sgate', b)])
                    for tl in range(n // 128):
                        tg = t0 // 128 + tl
                        ps, pk = proj_tm(512, 128, hb, hk, tl)
                        cp('dve', vpc[:, tg, 0, 0:64], ps[:, 0:64], [pk], ['vpc'])
                        cp('dve', vpc[:, tg, 1, 64:128], ps[:, 64:128], [pk], ['vpc'])
                    if b + 1 < 5:
                        prefetch_h(b + 1)
                    else:
                        pend_h[4] = (hb, hk)
                for dr in range(2):
                    oml = lbv[:, dr * 2 + pr:dr * 2 + pr + 1]
                    lbf = lbv[:, 4 + dr * 2 + pr:4 + dr * 2 + pr + 1]
                    border = [4, 0, 1, 2, 3] if dr == 0 else [4, 3, 2, 1, 0]
                    tiles = []
                    for bi, b in enumerate(border):
                        ntl = (BLK[b][1] - BLK[b][0]) // 128
                        tls = list(range(ntl)) if dr == 0 else list(range(ntl - 1, -1, -1))
                        for k_, tl in enumerate(tls):
                            tiles.append((bi, b, tl, k_ == 0))

                    def stage0_ops(bi, b):
                        bs = bi % 2
                        t0, t1 = BLK[b]
                        n = t1 - t0
                        nch = n // 16
                        ops_ = []
                        cell = {}

                        def o1():
                            hb, hk = get_h(b)
                            cell['ps'], cell['pk'] = proj_fm(128 + 128 * dr, b, hb, hk)
                            if bi + 1 < len(border):
                                prefetch_h(border[bi + 1])
                            elif dr == 0:
                                pend_h[4] = (hb, hk) if b == 4 else load_h(4)
                            act(t1b[:, :n], cell['ps'][:, :n], AF.Exp, [cell['pk']], ['t1b'], scale=-1.0)
                        ops_.append(o1)
                        ops_.append(lambda: act(t1b[:, :n], t1b[:, :n], AF.Ln, ['t1b'], ['t1b'], bias=1.0))
                        ops_.append(lambda: act(t1b[:, :n], t1b[:, :n], AF.Exp, ['t1b'], ['t1b'], scale=-1.0))
                        ops_.append(lambda: ts('dve', t1b[:, :n], t1b[:, :n], oml, lbf, ALU.mult, ALU.add, ['t1b', 'lbv'], ['t1b']))
                        ops_.append(lambda: ts('pool', t2b[:, :n], t1b[:, :n], -1.0, 1.0, ALU.mult, ALU.add, ['t1b'], ['t2b']))
                        ops_.append(lambda: act(t1b[:, :n], t1b[:, :n], AF.Ln, ['t1b'], ['t1b']))
                        ops_.append(lambda: S.op('dve', lambda e: e.tensor_tensor_scan(out=t3b[:, :n], data0=scanmask[:, :n], data1=t1b[:, :n],
                                                                                       initial=0.0, op0=ALU.mult, op1=ALU.add),
                                                 r=['t1b', 'cf32'], w=['t3b']))
                        c3v = t3b[:, :n].rearrange("p (c s) -> p c s", s=16)
                        if dr == 0:
                            cd, cdk = t3b, 't3b'
                            cend = c3v[:, :, 15:16]
                        else:
                            ops_.append(lambda: tt('dve', t4b[:, :n].rearrange("p (c s) -> p c s", s=16), c3v[:, :, 15:16].to_broadcast([128, nch, 16]),
                                                   c3v, ALU.subtract, ['t3b'], ['t4b']))
                            ops_.append(lambda: tt('dve', t4b[:, :n], t4b[:, :n], t1b[:, :n], ALU.add, ['t4b', 't1b'], ['t4b']))
                            cd, cdk = t4b, 't4b'
                            cend = t4b[:, :n].rearrange("p (c s) -> p c s", s=16)[:, :, 0:1]
                        ops_.append(lambda: act(ablk[bs][:, :nch].unsqueeze(2), cend, AF.Exp, [cdk], ['ablk%d' % bs]))
                        ops_.append(lambda: act(t5b[:, :n], cd[:, :n], AF.Exp, [cdk], ['t5b']))
                        ops_.append(lambda: act(t6b[:, :n], cd[:, :n], AF.Exp, [cdk], ['t6b'], scale=-1.0))
                        ops_.append(lambda: tt('dve', t7b[:, :n].rearrange("p (c s) -> p c s", s=16), cend.to_broadcast([128, nch, 16]),
                                               cd[:, :n].rearrange("p (c s) -> p c s", s=16), ALU.subtract, [cdk], ['t7b']))
                        ops_.append(lambda: act(t7b[:, :n], t7b[:, :n], AF.Exp, ['t7b'], ['t7b']))
                        ops_.append(lambda: tt('dve', qh[bs][:, :n], qs[:, t0:t1], t5b[:, :n], ALU.mult, [('qs', b), 't5b'], ['qh%d' % bs]))
                        ops_.append(lambda: tt('dve', kcbz[bs][0][0:64, :n], t2b[0:64, :n], t6b[0:64, :n], ALU.mult, ['t2b', 't6b'], ['kcb%d' % bs]))
                        ops_.append(lambda: tt('dve', kcbz[bs][1][64:128, :n], t2b[64:128, :n], t6b[64:128, :n], ALU.mult, ['t2b', 't6b'], ['kcb%d' % bs]))
                        ops_.append(lambda: tt('dve', kbb[bs][:, :n], t2b[:, :n], t7b[:, :n], ALU.mult, ['t2b', 't7b'], ['kbb%d' % bs]))
                        return ops_

                    pend = {'ops': [], 'per': 0}

                    def drain(k):
                        for _ in range(k):
                            if pend['ops']:
                                pend['ops'].pop(0)()

                    def hg_A(ti):
                        bi, b, tl, firstb = tiles[ti]
                        if firstb:
                            for f_ in stage0_ops(bi, b):
                                f_()
                        bs = bi % 2
                        x = ti % 2
                        x3 = ti % 3
                        t0 = BLK[b][0]
                        tg = t0 // 128 + tl
                        c0 = tl * 128
                        S.op('pe', lambda e, x=x, c0=c0, bs=bs: e.transpose(psb[x][:, 0:128], kbb[bs][:, c0:c0 + 128], ident_bf),
                             r=['kbb%d' % bs, 'cbf'], w=['psf%d' % (6 + x)])
                        cp('act', kbT[x][0][:, 0:64], psb[x][:, 0:64], ['psf%d' % (6 + x)], ['kbT%d' % x])
                        cp('act', kbT[x][1][:, 64:128], psb[x][:, 64:128], ['psf%d' % (6 + x)], ['kbT%d' % x])
                        cmv = cmfull.rearrange("p (a b) -> p a b", b=128)
                        for hh in range(2):
                            tt('pool', vblk[x][hh][:, :, hh * 64:(hh + 1) * 64],
                               vpc[:, tg, hh, hh * 64:(hh + 1) * 64].unsqueeze(1).to_broadcast([128, 8, 64]),
                               cmv[:, :, hh * 64:(hh + 1) * 64], ALU.mult, ['vpc', 'cf32'], ['vblk%d' % x])
                        for half in range(2):
                            pu = psf[2 + half]
                            for hh in range(2):
                                mm(pu[:, :], kbT[x][hh][:], vblk[x][hh][:, half * 4:(half + 1) * 4, :].rearrange("p a b -> p (a b)"),
                                   hh == 0, hh == 1, ['kbT%d' % x, 'vblk%d' % x], ['psf%d' % (2 + half)])
                            if dr == 0:
                                o_ap = U9[x3][:, :, 1 + half * 4:1 + half * 4 + 4].rearrange("p v j -> p j v")
                            else:
                                base = U9[x3][:, :, 8 - 4 * half:9 - 4 * half]
                                o_ap = bass.AP(base.tensor, base.offset, [list(base.ap[0]), [-1, 4], [9, 128]])
                            cp('act' if half == 0 else 'dve', o_ap, pu[:, :].rearrange("p (a b) -> p a b", b=128),
                               ['psf%d' % (2 + half)], ['U9%d' % x3])
                        for hh in range(2):
                            ps, pk = psf[hh], 'psf%d' % hh
                            mm(ps[:, 0:128], kcbz[bs][hh][:, c0:c0 + 128], qh[bs][:, c0:c0 + 128],
                               True, True, ['kcb%d' % bs, 'qh%d' % bs], [pk])
                            tt('dve', PA[x3][hh][:], ps[:, 0:128], Mdir[dr], ALU.mult, [pk, 'cf32'], ['PA%d%d' % (x3, hh)])

                    def hg_carry(ti):
                        bi, b, tl, firstb = tiles[ti]
                        bs = bi % 2
                        x = ti % 2
                        x3 = ti % 3
                        abase = ablk[bs][:, tl * 8 + (0 if dr == 0 else 7):tl * 8 + (0 if dr == 0 else 7) + 1]
                        a_in = bass.AP(abase.tensor, abase.offset, [list(abase.ap[0]), [0, 128], [1 if dr == 0 else -1, 8]])
                        act(A9[x][:, :, 1:9], a_in, AF.Copy, ['ablk%d' % bs], ['A9%d' % x])
                        if ti == 0:
                            memset('pool', U9[x3][:, :, 0:1], 0.0, ['U9%d' % x3])
                        else:
                            cp('act', U9[x3][:, :, 0:1], S9[1 - x][:, :, 8:9], ['S9%d' % (1 - x)], ['U9%d' % x3])

                    def hg_scan(ti):
                        x = ti % 2
                        x3 = ti % 3
                        S.op('dve', lambda e, x=x, x3=x3: e.tensor_tensor_scan(out=S9[x][:].rearrange("p v j -> p (v j)"),
                                                                               data0=A9[x][:].rearrange("p v j -> p (v j)"),
                                                                               data1=U9[x3][:].rearrange("p v j -> p (v j)"),
                                                                               initial=0.0, op0=ALU.mult, op1=ALU.add),
                             r=['A9%d' % x, 'U9%d' % x3], w=['S9%d' % x])

                    def hg_out(ti):
                        bi, b, tl, firstb = tiles[ti]
                        bs = bi % 2
                        x = ti % 2
                        x3 = ti % 3
                        t0 = BLK[b][0]
                        tg = t0 // 128 + tl
                        c0 = tl * 128
                        order = list(range(8)) if dr == 0 else list(range(7, -1, -1))
                        pI = psf[4]
                        for j, ci in enumerate(order):
                            mm(pI[:, ci * 16:(ci + 1) * 16], S9[x][:, :, j], qh[bs][:, c0 + ci * 16:c0 + (ci + 1) * 16], True, True,
                               ['S9%d' % x, 'qh%d' % bs], ['psf4'])
                        cp('act', oi[x][:], pI[:, 0:128], ['psf4'], ['oi%d' % x])
                        pOh = psf[5]
                        for hh in range(2):
                            mm(pOh[:, 0:128], vpc[:, tg, hh, :], PA[x3][hh][:], hh == 0, hh == 1, ['vpc', 'PA%d%d' % (x3, hh)], ['psf5'])
                        osl = osum[:, t0 + c0:t0 + c0 + 128]
                        if dr == 0:
                            tt('dve', osl, pOh[:, 0:128], oi[x][:], ALU.add, ['psf5', 'oi%d' % x], [('osum', b)])
                        else:
                            tt('dve', oi[x][:], pOh[:, 0:128], oi[x][:], ALU.add, ['psf5', 'oi%d' % x], ['oi%d' % x])
                            tt('pool', osl, osl, oi[x][:], ALU.add, ['oi%d' % x, ('osum', b)], [('osum', b)])

                    ntile = len(tiles)
                    for t_ in range(-1, ntile + 1):
                        if 0 <= t_ - 1 < ntile:
                            hg_scan(t_ - 1)
                        if 0 <= t_ + 1 < ntile:
                            hg_A(t_ + 1)
                        if 0 <= t_ < ntile:
                            hg_carry(t_)
                        if 0 <= t_ - 1 < ntile:
                            hg_out(t_ - 1)
                for b in range(nblk):
                    t0, t1 = BLK[b]
                    n = t1 - t0
                    act(sqh[:, :n], osum[:, t0:t1], AF.Square, [('osum', b)], ['qh0'])
                    p2, p2k = nextps()
                    mm(p2[:, :n], onesbd_bf, sqh[:, :n], True, True, ['qh0', 'cbf'], [p2k])
                    act(rs[:, :n], p2[:, :n], AF.Ln, [p2k], ['t1b'], scale=1.0 / 64, bias=EPS)
                    act(rs[:, :n], rs[:, :n], AF.Exp, ['t1b'], ['t1b'], scale=-0.5)
                    stt('dve', rs[:, :n], osum[:, t0:t1], vecs[:, 20:21], rs[:, :n], ALU.mult, ALU.mult, [('osum', b), 't1b', 'vecs'], ['t1b'])
                    tt('dve', mixu[:, 0, t0:t1], rs[:, :n], sgate[:, t0:t1], ALU.mult, ['t1b', ('sgate', b)], [('mixu', b)])
                if 'mixC%d' % pr in dbg and l == 0:
                    dump('mixC%d' % pr, mixu[:, 0, :], [('mixu', b) for b in range(5)], [128, T], BF16)
                    dump('osum%d' % pr, osum[:, :], [('osum', b) for b in range(5)], [128, T], F32)
                wout_partial(1, 6 + pr, range(nblk))
            S.barrier()
        stage(4)
        if 'xmid' in dbg and l == 0:
            dump('xmid', XT[:, :, :], [('XT', b) for b in range(5)], [128, 8, T], F32)
            S.barrier()
        stm.close()

        with contextlib.ExitStack() as st2:
            def sb2(name, shape, dt=F32):
                return st2.enter_context(nc.sbuf_tensor(name + "_m%d" % l, list(shape), dt))
            h2T = sb2("h2T", [128, 8, T], BF16)
            combT = sb2("combT", [32, T])
            wrt = sb2("wrt", [128, 8, 36])
            brt = sb2("brt", [128, 36])
            dma('sp', wrt[:], wr_d[l].rearrange("(kc p) n -> p kc n", p=128), [], ['wrt'], 'c3')
            dma('sp', brt[:], brb_d[l], [], ['brt'], 'c3')
            with contextlib.ExitStack() as st3:
                def sb3(name, shape, dt=F32):
                    return st3.enter_context(nc.sbuf_tensor(name + "_r%d" % l, list(shape), dt))
                tmp2 = [sb3("ntmp%d" % i, [128, 8, 512]) for i in range(2)]
                sqb = sb3("sqb", [128, 8, 512], BF16)
                rstd2 = [sb3("rstd%d" % i, [128, 512]) for i in range(2)]
                lg = sb3("lg", [128, 4, 36])
                gmax = sb3("gmax", [128, 4])
                gsh = sb3("gsh", [128, 4, 4])
                gsum = sb3("gsum", [128, 4])
                ghot = sb3("ghot", [128, 4, 4])
                msk = sb3("msk", [128, 4, 4, 8])
                ing = sb3("ing", [128, 4, 8])
                ing2 = sb3("ing2", [128, 4, 8])
                m1 = sb3("m1", [128, 4])
                m2 = sb3("m2", [128, 4])
                hot1 = sb3("hot1", [128, 4, 8])
                hot2 = sb3("hot2", [128, 4, 8])
                w1 = sb3("w1", [128, 4])
                w2 = sb3("w2", [128, 4])
                ce = sb3("ce", [128, 4, 8])
                comb = sb3("comb", [128, 4, 4, 8])
                norm_block(0, (A2, 'A2'), 3, tmp2[0], sqb, rstd2[0], '0', '')
                for b in range(nblk):
                    t0, t1 = BLK[b]
                    n = t1 - t0
                    nt = n // 128
                    tmp = tmp2[b % 2]
                    ktm = 'ntmp%d' % (b % 2)
                    if b + 1 < nblk:
                        nb_ = (b + 1) % 2
                        norm_block(b + 1, (A2, 'A2'), 3, tmp2[nb_], sqb, rstd2[nb_], str(nb_), '')
                    cp('pool', h2T[:, :, t0:t1], tmp[:, :, :n], [ktm], [('h2T', b)])
                    pR = psf[4]
                    for tl in range(nt):
                        for kc in range(8):
                            mm(pR[:, tl * 36:(tl + 1) * 36], tmp[:, kc, tl * 128:(tl + 1) * 128], wrt[:, kc, :], kc == 0, kc == 7,
                               [ktm, 'wrt'], ['pR'])
                    R = ['rt']
                    tt('dve', lg[:, :nt, :], pR[:, 0:nt * 36].rearrange("p (a b) -> p a b", b=36),
                       brt[:].unsqueeze(1).to_broadcast([128, nt, 36]), ALU.add, ['pR', 'brt'], R)
                    G = lg[:, :nt, 0:4]
                    S.op('dve', lambda e, nt=nt, G=G: e.tensor_reduce(out=gmax[:, :nt], in_=G, axis=AX.X, op=ALU.max), r=R, w=R)
                    tt('dve', gsh[:, :nt, :], G, gmax[:, :nt].unsqueeze(2).to_broadcast([128, nt, 4]), ALU.subtract, R, R)
                    tt('dve', ghot[:, :nt, :], G, gmax[:, :nt].unsqueeze(2).to_broadcast([128, nt, 4]), ALU.is_ge, R, R)
                    act(gsh[:, :nt, :], gsh[:, :nt, :], AF.Exp, R, R)
                    S.op('dve', lambda e, nt=nt: e.tensor_reduce(out=gsum[:, :nt], in_=gsh[:, :nt, :], axis=AX.X, op=ALU.add), r=R, w=R)
                    recip(gsum[:, :nt], gsum[:, :nt], R, R)
                    E4 = lg[:, :nt, 4:36].rearrange("p t (g e) -> p t g e", e=8)
                    tt('dve', msk[:, :nt], E4, ghot[:, :nt, :].unsqueeze(3).to_broadcast([128, nt, 4, 8]), ALU.mult, R, R)
                    S.op('dve', lambda e, nt=nt: e.tensor_reduce(out=ing[:, :nt, :], in_=msk[:, :nt].rearrange("p t g e -> p t e g"),
                                                                 axis=AX.X, op=ALU.add), r=R, w=R)
                    S.op('dve', lambda e, nt=nt: e.tensor_reduce(out=m1[:, :nt], in_=ing[:, :nt, :], axis=AX.X, op=ALU.max), r=R, w=R)
                    tt('dve', hot1[:, :nt, :], ing[:, :nt, :], m1[:, :nt].unsqueeze(2).to_broadcast([128, nt, 8]), ALU.is_ge, R, R)
                    stt('dve', ing2[:, :nt, :], hot1[:, :nt, :], -1e30, ing[:, :nt, :], ALU.mult, ALU.add, R, R)
                    S.op('dve', lambda e, nt=nt: e.tensor_reduce(out=m2[:, :nt], in_=ing2[:, :nt, :], axis=AX.X, op=ALU.max), r=R, w=R)
                    tt('dve', hot2[:, :nt, :], ing2[:, :nt, :], m2[:, :nt].unsqueeze(2).to_broadcast([128, nt, 8]), ALU.is_ge, R, R)
                    tt('dve', w2[:, :nt], m2[:, :nt], m1[:, :nt], ALU.subtract, R, R)
                    act(w2[:, :nt], w2[:, :nt], AF.Exp, R, R)
                    ts('dve', w1[:, :nt], w2[:, :nt], 1.0, None, ALU.add, None, R, R)
                    recip(w1[:, :nt], w1[:, :nt], R, R)
                    tt('dve', w2[:, :nt], w2[:, :nt], w1[:, :nt], ALU.mult, R, R)
                    tt('dve', w1[:, :nt], w1[:, :nt], gsum[:, :nt], ALU.mult, R, R)
                    tt('dve', w2[:, :nt], w2[:, :nt], gsum[:, :nt], ALU.mult, R, R)
                    tt('dve', ce[:, :nt, :], hot1[:, :nt, :], w1[:, :nt].unsqueeze(2).to_broadcast([128, nt, 8]), ALU.mult, R, R)
                    tt('dve', hot2[:, :nt, :], hot2[:, :nt, :], w2[:, :nt].unsqueeze(2).to_broadcast([128, nt, 8]), ALU.mult, R, R)
                    tt('dve', ce[:, :nt, :], ce[:, :nt, :], hot2[:, :nt, :], ALU.add, R, R)
                    tt('dve', comb[:, :nt], ghot[:, :nt, :].unsqueeze(3).to_broadcast([128, nt, 4, 8]),
                       ce[:, :nt, :].unsqueeze(2).to_broadcast([128, nt, 4, 8]), ALU.mult, R, R)
                    pX = psf[5]
                    for tl in range(nt):
                        mm(pX[0:32, tl * 128:(tl + 1) * 128], comb[:, tl].rearrange("p g e -> p (g e)"), ident_f, True, True,
                           R + ['cf32'], ['pX'])
                    cp('act', combT[:, t0:t1], pX[0:32, 0:n], ['pX'], [('combT', b)])
                if 'combT' in dbg and l == 0:
                    dump('combT', combT[:, :], [('combT', b) for b in range(5)], [32, T], F32)
                    dump('h2T', h2T[:, :, :], [('h2T', b) for b in range(5)], [128, 8, T], BF16)
            dma('sp', comb_d[:, 0:BLK[nblk - 1][1]], combT[:, 0:BLK[nblk - 1][1]], [('combT', b) for b in range(nblk)], ['combd'], 'cbd')
            S.barrier()
            stage(5)
            we = [sb2("we%d" % i, [128, 6144], BF16) for i in range(2)]
            hid = [sb2("hid%d" % i, [128, 2, 512], BF16) for i in range(2)]
            cbcE = [sb2("cbcE%d" % i, [128, T]) for i in range(2)]
            sgs = [sb2("sgs%d" % i, [128, 512], BF16) for i in range(4)]
            ut = [sb2("ut%d" % i, [128, 512]) for i in range(4)]
            def load_exp(ex):
                wt = we[ex % 2]
                wk = 'we%d' % (ex % 2)
                dma('pool', wt[:, 0:2048], weg_d[l, ex], [], [wk], wk)
                dma('pool', wt[:, 2048:4096], weu_d[l, ex], [], [wk], wk)
                dma('pool', wt[:, 4096:6144], wed_d[l, ex], [], [wk], wk)

            def load_cbc(ex):
                i_ = ex % 2
                dma('sp', cbcE[i_][:, :], comb_d[ex:ex + 1, :].partition_broadcast(128), ['combd'], ['cbcE%d' % i_], 'cbl%d' % i_)

            its = [(ex, b) for ex in range(32) for b in range(nblk)]

            def moe_A(i):
                ex, b = its[i]
                wt = we[ex % 2]
                wk = 'we%d' % (ex % 2)
                wg = wt[:, 0:2048].rearrange("p (k f) -> p k f", f=256)
                wu = wt[:, 2048:4096].rearrange("p (k f) -> p k f", f=256)
                t0, t1 = BLK[b]
                n = t1 - t0
                i2 = i % 2
                for fc in range(2):
                    pG, pGk = psf[fc], 'psf%d' % fc
                    pU, pUk = psf[2 + fc], 'psf%d' % (2 + fc)
                    for kc in range(8):
                        mm(pG[:, :n], wg[:, kc, fc * 128:(fc + 1) * 128], h2T[:, kc, t0:t1], kc == 0, kc == 7, [wk, ('h2T', b)], [pGk])
                    for kc in range(8):
                        mm(pU[:, :n], wu[:, kc, fc * 128:(fc + 1) * 128], h2T[:, kc, t0:t1], kc == 0, kc == 7, [wk, ('h2T', b)], [pUk])
                    i3 = i2 * 2 + fc
                    act(sgs[i3][:, :n], pG[:, :n], AF.Silu, [pGk], ['sgs%d' % i3])
                    tt('dve', ut[i3][:, :n], pU[:, :n], cbcE[ex % 2][:, t0:t1], ALU.mult, [pUk, 'cbcE%d' % (ex % 2)], ['ut%d' % i3])
                    tt('pool', hid[i2][:, fc, :n], ut[i3][:, :n], sgs[i3][:, :n], ALU.mult, ['ut%d' % i3, 'sgs%d' % i3], ['hid%d' % i2])

            def moe_B(i):
                ex, b = its[i]
                wt = we[ex % 2]
                wk = 'we%d' % (ex % 2)
                wd = wt[:, 4096:6144].rearrange("p (k f) -> p k f", f=1024)
                t0, t1 = BLK[b]
                n = t1 - t0
                col = 0 if b < 4 else 1
                i2 = i % 2
                for dc in range(8):
                    bd_ = 4 + dc % 4
                    pD = psf[bd_]
                    for fc in range(2):
                        mm(pD[:, :n], wd[:, fc, dc * 128:(dc + 1) * 128], hid[i2][:, fc, :n], fc == 0, fc == 1, [wk, 'hid%d' % i2], ['psf%d' % bd_])
                    stt('dve', XT[:, dc, t0:t1], pD[:, :n], modv(5, dc, col), XT[:, dc, t0:t1], ALU.mult, ALU.add,
                        ['psf%d' % bd_, 'modT'] + XTk(b), XTk(b))

            load_exp(0)
            load_exp(1)
            load_cbc(0)
            load_cbc(1)
            nit = len(its)
            for t_ in range(nit + 1):
                if t_ < nit:
                    moe_A(t_)
                if t_ >= 1:
                    moe_B(t_ - 1)
                if t_ < nit and its[t_][1] == 0 and 1 <= its[t_][0] < 31:
                    load_exp(its[t_][0] + 1)
                    load_cbc(its[t_][0] + 1)
        S.barrier()
        if 'xl0' in dbg and l == 0:
            dump('xl0', XT[:, :, :], [('XT', b) for b in range(5)], [128, 8, T], F32)

    try:
        for l in range(n_layers):
            layer(l)
    except _Stop:
        stopped = True
    else:
        stopped = False
    ov = outT.rearrange("(c p) t -> p c t", p=128)
    for c in range(8):
        dma('sp', ov[:, c, :], XT[:, c, 0:SL], [('XT', b) for b in range(4)], ['out'], 'o%d' % (c % 4))
    S.op('sp', None, r=['out'] + ['dbg_' + k for k in dbg_out])
    S.emit()
    if not stopped:
        st.close()
    return nc, dbg_out


def _consts():
    p = np.arange(128)
    ident = np.eye(128, dtype=np.float32)
    swap = np.zeros((128, 128), np.float32)
    swap[p, p ^ 1] = 1.0
    onesbd = (p[:, None] // 64 == p[None, :] // 64).astype(np.float32)
    ones = np.ones((128, 128), np.float32)
    oh0 = np.broadcast_to((p[None, :] // 64 == 0), (128, 128)).astype(np.float32)
    oh1 = np.broadcast_to((p[None, :] // 64 == 1), (128, 128)).astype(np.float32)
    cbf = np.concatenate([ident, swap, onesbd, ones, oh0, oh1], axis=1)
    ck = p % 64
    cq = np.arange(64)
    cs = np.clip(cq - 8, 0, 48)
    ok = (ck[:, None] >= cs[None, :]) & (ck[:, None] < cs[None, :] + 16)
    colmask = np.where(ok, 0.0, -1e30).astype(np.float32)
    same = (p[:, None] // 16 == p[None, :] // 16)
    Mf = (same & (p[:, None] <= p[None, :])).astype(np.float32)
    Mb = (same & (p[:, None] >= p[None, :])).astype(np.float32)
    bd = onesbd
    cm = (p[:, None] // 16 == np.arange(8)[None, :]).astype(np.float32)
    cmfull = np.repeat(cm[:, :, None], 128, axis=2).reshape(128, 1024)
    sm = np.ones((128, 512), np.float32)
    sm[:, ::16] = 0.0
    cf32 = np.concatenate([ident, colmask, Mf, Mb, bd, cmfull, sm], axis=1)
    sel = np.zeros((32, 32, 128), np.float32)
    for e in range(32):
        sel[e, e, :] = 1.0
    sel = sel.reshape(32, 4096)
    t = np.arange(SL)
    row = (t // 64).astype(np.float32)
    colp = (t % 64).astype(np.float32)
    half = 32
    inv = (np.float32(10000.0) ** (-np.arange(0, half, 2, dtype=np.float32) / np.float32(half))).astype(np.float32)
    ang = np.concatenate([row[:, None] * inv, colp[:, None] * inv], axis=-1).astype(np.float32)
    cos = np.cos(ang).astype(np.float32)
    sin = np.sin(ang).astype(np.float32)
    d = p % 64
    cosT = cos[:, d // 2].T
    sgn = np.where(d % 2 == 0, -1.0, 1.0).astype(np.float32)
    sinT = sin[:, d // 2].T * sgn[:, None]
    rope = np.stack([cosT, sinT], axis=1).astype(np.float32)
    return (np.ascontiguousarray(cbf), np.ascontiguousarray(cf32), np.ascontiguousarray(sel), np.ascontiguousarray(rope))


def make_in_maps(x, c, ctx, c_ctx, w_ada, b_ada, norm1_g, w_in, na_q_norm, na_k_norm, na_rpb,
                 gqa_q_norm, gqa_k_norm, hgrn_lb, hgrn_o_norm, w_out, norm2_g, w_route_group,
                 b_route_group, w_route_expert, b_route_expert, w_exp_gate, w_exp_up, w_exp_down):
    f = lambda a: np.ascontiguousarray(np.asarray(a, dtype=np.float32))
    x, c, ctx, c_ctx = f(x), f(c), f(ctx), f(c_ctx)
    cbf, cf32, sel, rope = _consts()
    b_adaT = f(np.asarray(b_ada).reshape(NL, 48, 128).transpose(0, 2, 1))
    rep2 = lambda v: np.concatenate([np.asarray(v), np.asarray(v)], axis=1)
    vecs = np.concatenate([
        np.asarray(norm1_g).reshape(NL, 8, 128).transpose(0, 2, 1),
        np.asarray(norm2_g).reshape(NL, 8, 128).transpose(0, 2, 1),
        rep2(na_q_norm)[:, :, None], rep2(na_k_norm)[:, :, None], rep2(gqa_q_norm)[:, :, None],
        rep2(gqa_k_norm)[:, :, None], rep2(hgrn_o_norm)[:, :, None]], axis=2)
    vecs = f(vecs)
    lbraw = f(np.asarray(hgrn_lb).reshape(NL, 2, 2, 128).transpose(3, 0, 1, 2).reshape(128, 8))
    ck = np.arange(128) % 64
    cq = np.arange(64)
    dc = np.clip(ck[:, None] - cq[None, :] + 15, 0, 30)
    nab = np.asarray(na_rpb)[:, :, :, dc]
    nab = f(nab.transpose(0, 3, 1, 2, 4).reshape(NL, 128, 3840))
    wr = f(np.concatenate([np.asarray(w_route_group), np.asarray(w_route_expert)], axis=2))
    br = np.concatenate([np.asarray(b_route_group), np.asarray(b_route_expert)], axis=1)
    brb = f(np.broadcast_to(br[:, None, :], (NL, 128, 36)))
    shared = dict(cbf=cbf, cf32=cf32, sel=sel, rope=rope, w_ada=f(w_ada), b_adaT=b_adaT, vecs=vecs, lbraw=lbraw,
                  w_in=f(w_in), nab=nab, w_out=f(w_out), wr=wr, brb=brb,
                  w_eg=f(np.asarray(w_exp_gate).reshape(NL, 32, 8, 128, 256).transpose(0, 1, 3, 2, 4).reshape(NL, 32, 128, 2048)),
                  w_eu=f(np.asarray(w_exp_up).reshape(NL, 32, 8, 128, 256).transpose(0, 1, 3, 2, 4).reshape(NL, 32, 128, 2048)),
                  w_ed=f(np.asarray(w_exp_down).reshape(NL, 32, 2, 128, 1024).transpose(0, 1, 3, 2, 4).reshape(NL, 32, 128, 2048)))
    maps = []
    for b in range(x.shape[0]):
        xT = np.ascontiguousarray(np.concatenate([x[b].T, ctx[b].T], axis=1))
        cv = np.stack([c[b].reshape(8, 128).T, c_ctx.reshape(8, 128).T], axis=2).reshape(128, 16)
        m = dict(shared)
        m['xT_in'] = xT
        m['cvec'] = np.ascontiguousarray(cv.astype(np.float32))
        maps.append(m)
    return maps


_NC_CACHE = {}


def kernel(**inputs):
    maps = make_in_maps(**inputs)
    if 'nc' not in _NC_CACHE:
        _NC_CACHE['nc'] = build_nc()[0]
    nc = _NC_CACHE['nc']
    res = run_bass_kernel_spmd(nc, maps, core_ids=list(range(8)))
    out = np.stack([np.ascontiguousarray(r["outT"].T) for r in res.results], axis=0)
    return out.astype(np.float32)
```
